# Optimizing a Trainium2 kernel written in Bass

```python
import math
import jax, jax.numpy as jnp
from jax import lax
import numpy as np


D_MODEL = 1024
BATCH = 2
SEQ = 8192
DEPTH = 2

N_A_LAYERS = DEPTH // 2
N_B_LAYERS = DEPTH - N_A_LAYERS
ALPHA = (2.0 * DEPTH) ** 0.25
BETA = (8.0 * DEPTH) ** -0.25
LN_EPS = 1e-5
ROPE_THETA = 10000.0
NEG = -1e30

RET_HEADS = 4
RET_DK = D_MODEL // RET_HEADS
RET_DV = 2 * RET_DK
RET_CHUNK = 128

NSA_HEADS = 16
NSA_GROUPS = 4
NSA_REP = NSA_HEADS // NSA_GROUPS
NSA_DH = D_MODEL // NSA_HEADS
KV_W = NSA_GROUPS * NSA_DH
CMP_LEN = 32
CMP_STRIDE = 16
CMP_HIDDEN = 256
SLC_BLOCK = 64
SLC_TOPK = 16
WINDOW = 512
Q_BLOCK = 128
FORCE_BONUS = 1e3

N_EXPERTS = 16
N_EXPERT_GROUPS = 4
EXPERTS_PER_GROUP = N_EXPERTS // N_EXPERT_GROUPS
TOP_K = 2
D_EXPERT = 512

kernel_name = 'hybrid_retnet_nsa_moe'


def layer_norm(x, g, b):
    xf = x.astype(jnp.float32)
    mu = xf.mean(-1, keepdims=True)
    var = jnp.square(xf - mu).mean(-1, keepdims=True)
    y = (xf - mu) * lax.rsqrt(var + LN_EPS) * g.astype(jnp.float32) + b.astype(jnp.float32)
    return y.astype(x.dtype)


def rope(x, pos):
    d = x.shape[-1]
    inv = ROPE_THETA ** (-jnp.arange(0, d, 2, dtype=jnp.float32) / d)
    ang = pos.astype(jnp.float32)[:, None] * inv[None, :]
    cos = jnp.cos(ang)[None, :, None, :]
    sin = jnp.sin(ang)[None, :, None, :]
    xf = x.astype(jnp.float32)
    x1, x2 = xf[..., : d // 2], xf[..., d // 2:]
    return jnp.concatenate([x1 * cos - x2 * sin, x2 * cos + x1 * sin], -1).astype(x.dtype)


def masked_softmax(s, mask):
    p = jax.nn.softmax(jnp.where(mask, s, NEG), axis=-1)
    return jnp.where(mask, p, 0.0)


def retention(h, w_in, w_out):
    B, S, _ = h.shape
    dt = h.dtype
    H, dk, dv, C = RET_HEADS, RET_DK, RET_DV, RET_CHUNK
    proj = h @ w_in
    q, k, v, g = jnp.split(proj, [H * dk, 2 * H * dk, 2 * H * dk + H * dv], axis=-1)
    pos = jnp.arange(S)
    q = rope(q.reshape(B, S, H, dk), pos)
    k = rope(k.reshape(B, S, H, dk), pos) * (dk ** -0.5)
    v = v.reshape(B, S, H, dv)
    nC = S // C

    def chunks(t):
        return t.reshape(B, nC, C, H, -1).transpose(0, 3, 1, 2, 4)

    qc, kc, vc = chunks(q), chunks(k), chunks(v)
    log_gamma = jnp.log1p(-(2.0 ** (-5.0 - jnp.arange(H, dtype=jnp.float32))))
    idx = jnp.arange(C, dtype=jnp.float32)
    diff = idx[:, None] - idx[None, :]
    decay_mask = jnp.where(diff >= 0, jnp.exp(log_gamma[:, None, None] * jnp.maximum(diff, 0.0)), 0.0).astype(dt)
    inner = jnp.einsum('bhncd,bhnmd->bhncm', qc, kc) * decay_mask[None, :, None]
    o_inner = jnp.einsum('bhncm,bhnme->bhnce', inner, vc)
    q_decay = jnp.exp(log_gamma[:, None] * (idx[None, :] + 1.0)).astype(dt)
    k_decay = jnp.exp(log_gamma[:, None] * (C - 1.0 - idx[None, :])).astype(dt)
    chunk_decay = jnp.exp(log_gamma * C).astype(dt)

    def step(state, xs):
        q_i, k_i, v_i = xs
        o = jnp.einsum('bhcd,bhde->bhce', q_i * q_decay[None, :, :, None], state)
        state = state * chunk_decay[None, :, None, None] + jnp.einsum(
            'bhcd,bhce->bhde', k_i * k_decay[None, :, :, None], v_i)
        return state, o

    state0 = jnp.zeros((B, H, dk, dv), dt)
    _, o_cross = lax.scan(step, state0, (qc.transpose(2, 0, 1, 3, 4),
                                         kc.transpose(2, 0, 1, 3, 4),
                                         vc.transpose(2, 0, 1, 3, 4)))
    o = o_inner + o_cross.transpose(1, 2, 0, 3, 4)
    o = o.transpose(0, 2, 3, 1, 4).reshape(B, S, H, dv)
    of = o.astype(jnp.float32)
    mu = of.mean(-1, keepdims=True)
    var = jnp.square(of - mu).mean(-1, keepdims=True)
    o = ((of - mu) * lax.rsqrt(var + LN_EPS)).reshape(B, S, H * dv).astype(dt)
    return (jax.nn.silu(g) * o) @ w_out


def nsa_shared_kv(h, w_kv, cmp_pe_k, cmp_pe_v, cmp_k_w1, cmp_k_w2, cmp_v_w1, cmp_v_w2):
    B, S, _ = h.shape
    G, dh = NSA_GROUPS, NSA_DH
    pos = jnp.arange(S)
    parts = jnp.split(h @ w_kv, 6, axis=-1)
    kc, vc, ks, vs, kw, vw = [p.reshape(B, S, G, dh) for p in parts]
    kc, ks, kw = rope(kc, pos), rope(ks, pos), rope(kw, pos)
    n_cmp = (S - CMP_LEN) // CMP_STRIDE + 1
    cidx = jnp.arange(n_cmp)[:, None] * CMP_STRIDE + jnp.arange(CMP_LEN)[None, :]

    def compress(t, pe, w1, w2):
        blk = t[:, cidx] + pe[None, None, :, None, :]
        blk = blk.transpose(0, 3, 1, 2, 4).reshape(B, G, n_cmp, CMP_LEN * dh)
        return jax.nn.gelu(blk @ w1) @ w2

    k_cmp = compress(kc, cmp_pe_k, cmp_k_w1, cmp_k_w2)
    v_cmp = compress(vc, cmp_pe_v, cmp_v_w1, cmp_v_w2)
    nS = S // SLC_BLOCK

    def to_blocks(t):
        return t.transpose(0, 2, 1, 3).reshape(B, G, nS, SLC_BLOCK, dh)

    def pad_win(t):
        return jnp.pad(t.transpose(0, 2, 1, 3), ((0, 0), (0, 0), (WINDOW, 0), (0, 0)))

    return (k_cmp, v_cmp, to_blocks(ks), to_blocks(vs), pad_win(kw), pad_win(vw))


def nsa_attention(h, w_in, w_out, shared):
    k_cmp, v_cmp, k_slc, v_slc, k_win, v_win = shared
    B, S, _ = h.shape
    dt = h.dtype
    H, G, R, dh = NSA_HEADS, NSA_GROUPS, NSA_REP, NSA_DH
    proj = h @ w_in
    q = rope(proj[..., : H * dh].reshape(B, S, H, dh), jnp.arange(S)) * (dh ** -0.5)
    gates = jax.nn.sigmoid(proj[..., H * dh:].astype(jnp.float32)).reshape(B, S, H, 3)
    nQ = S // Q_BLOCK
    q_blocks = q.reshape(B, nQ, Q_BLOCK, G, R, dh).transpose(1, 0, 3, 4, 2, 5)
    g_blocks = gates.reshape(B, nQ, Q_BLOCK, G, R, 3).transpose(1, 0, 3, 4, 2, 5)
    n_cmp = k_cmp.shape[2]
    nS = k_slc.shape[2]
    top_k = min(SLC_TOPK, nS)
    ci = jnp.arange(n_cmp)
    sj = jnp.arange(nS)
    cmp_end = ci * CMP_STRIDE + CMP_LEN - 1
    overlap = ((ci[:, None] * CMP_STRIDE < (sj[None, :] + 1) * SLC_BLOCK)
               & (ci[:, None] * CMP_STRIDE + CMP_LEN > sj[None, :] * SLC_BLOCK)).astype(jnp.float32)
    gather_blocks = jax.vmap(jax.vmap(lambda kb, ix: kb[ix]))

    def block(xs):
        i, qb, gb = xs
        t = i * Q_BLOCK + jnp.arange(Q_BLOCK)
        s = jnp.einsum('bgrqd,bgnd->bgrqn', qb, k_cmp).astype(jnp.float32)
        p_cmp = masked_softmax(s, cmp_end[None, :] <= t[:, None])
        o_cmp = jnp.einsum('bgrqn,bgnd->bgrqd', p_cmp.astype(dt), v_cmp)
        imp = jnp.einsum('bgrqn,ns->bgqs', p_cmp, overlap)
        valid = sj[None, :] * SLC_BLOCK <= t[:, None]
        cur = t // SLC_BLOCK
        forced = (sj[None, :] == 0) | (sj[None, :] == cur[:, None]) | (sj[None, :] == cur[:, None] - 1)
        score = jnp.where(valid, imp + jnp.where(forced, FORCE_BONUS, 0.0), NEG)
        _, sel = lax.top_k(score, top_k)
        ks = gather_blocks(k_slc, sel).reshape(B, G, Q_BLOCK, top_k * SLC_BLOCK, dh)
        vs = gather_blocks(v_slc, sel).reshape(B, G, Q_BLOCK, top_k * SLC_BLOCK, dh)
        key_pos = (sel[..., None] * SLC_BLOCK + jnp.arange(SLC_BLOCK)).reshape(B, G, Q_BLOCK, top_k * SLC_BLOCK)
        s = jnp.einsum('bgrqd,bgqkd->bgrqk', qb, ks).astype(jnp.float32)
        p = masked_softmax(s, (key_pos <= t[:, None])[:, :, None])
        o_slc = jnp.einsum('bgrqk,bgqkd->bgrqd', p.astype(dt), vs)
        kw = lax.dynamic_slice_in_dim(k_win, i * Q_BLOCK, Q_BLOCK + WINDOW, axis=2)
        vw = lax.dynamic_slice_in_dim(v_win, i * Q_BLOCK, Q_BLOCK + WINDOW, axis=2)
        wpos = i * Q_BLOCK - WINDOW + jnp.arange(Q_BLOCK + WINDOW)
        dist = t[:, None] - wpos[None, :]
        wmask = (dist >= 0) & (dist < WINDOW) & (wpos[None, :] >= 0)
        s = jnp.einsum('bgrqd,bgkd->bgrqk', qb, kw).astype(jnp.float32)
        p = masked_softmax(s, wmask)
        o_win = jnp.einsum('bgrqk,bgkd->bgrqd', p.astype(dt), vw)
        o = gb[..., 0:1] * o_cmp + gb[..., 1:2] * o_slc + gb[..., 2:3] * o_win
        return o.astype(dt)

    o = lax.map(block, (jnp.arange(nQ), q_blocks, g_blocks))
    o = o.transpose(1, 0, 4, 2, 3, 5).reshape(B, S, H * dh)
    return o @ w_out


def moe(h, router_w, router_b, w_gate, w_up, w_down):
    B, S, D = h.shape
    x = h.reshape(-1, D)
    T = x.shape[0]
    aff = jax.nn.sigmoid((x @ router_w).astype(jnp.float32))
    sel_score = aff + router_b.astype(jnp.float32)
    grp_score = lax.top_k(sel_score.reshape(T, N_EXPERT_GROUPS, EXPERTS_PER_GROUP), TOP_K)[0].sum(-1)
    best = jnp.argmax(grp_score, axis=-1)
    in_group = (jnp.arange(N_EXPERTS) // EXPERTS_PER_GROUP)[None, :] == best[:, None]
    _, top_idx = lax.top_k(jnp.where(in_group, sel_score, NEG), TOP_K)
    chosen = jax.nn.one_hot(top_idx, N_EXPERTS, dtype=jnp.float32).sum(1)
    w = aff * chosen
    w = (w / w.sum(-1, keepdims=True)).astype(x.dtype)
    y = jnp.zeros_like(x)
    for e in range(N_EXPERTS):
        he = (jax.nn.silu(x @ w_gate[e]) * (x @ w_up[e])) @ w_down[e]
        y = y + w[:, e:e + 1] * he
    return y.reshape(B, S, D)


def setup_inputs(seed: int = 0) -> dict:
    key = jax.random.key(seed)
    ks = jax.random.split(key, 32)
    f32 = jnp.float32
    D = D_MODEL

    def nrm(k, shape, scale):
        return jax.random.normal(k, shape, f32) * scale

    x = nrm(ks[0], (BATCH, SEQ, D), 1.0)
    ret_w_in = jnp.concatenate([
        nrm(ks[1], (N_A_LAYERS, D, 2 * RET_HEADS * RET_DK), D ** -0.5),
        nrm(ks[2], (N_A_LAYERS, D, RET_HEADS * RET_DV), D ** -0.5 * BETA),
        nrm(ks[3], (N_A_LAYERS, D, RET_HEADS * RET_DV), D ** -0.5)], axis=-1)
    ret_w_out = nrm(ks[4], (N_A_LAYERS, RET_HEADS * RET_DV, D), (RET_HEADS * RET_DV) ** -0.5 * BETA)
    kv_k = nrm(ks[5], (3, D, KV_W), D ** -0.5)
    kv_v = nrm(ks[6], (3, D, KV_W), D ** -0.5 * BETA)
    nsa_w_kv = jnp.stack([kv_k, kv_v], 1).transpose(2, 0, 1, 3).reshape(D, 6 * KV_W)
    cmp_pe_k = nrm(ks[7], (CMP_LEN, NSA_DH), 0.1)
    cmp_pe_v = nrm(ks[8], (CMP_LEN, NSA_DH), 0.1)
    cmp_k_w1 = nrm(ks[9], (CMP_LEN * NSA_DH, CMP_HIDDEN), (CMP_LEN * NSA_DH) ** -0.5)
    cmp_k_w2 = nrm(ks[10], (CMP_HIDDEN, NSA_DH), CMP_HIDDEN ** -0.5)
    cmp_v_w1 = nrm(ks[11], (CMP_LEN * NSA_DH, CMP_HIDDEN), (CMP_LEN * NSA_DH) ** -0.5)
    cmp_v_w2 = nrm(ks[12], (CMP_HIDDEN, NSA_DH), CMP_HIDDEN ** -0.5)
    nsa_w_in = nrm(ks[13], (N_B_LAYERS, D, NSA_HEADS * NSA_DH + 3 * NSA_HEADS), D ** -0.5)
    nsa_w_out = nrm(ks[14], (N_B_LAYERS, NSA_HEADS * NSA_DH, D), (NSA_HEADS * NSA_DH) ** -0.5 * BETA)
    router_w = nrm(ks[15], (D, N_EXPERTS), D ** -0.5)
    router_b = nrm(ks[16], (N_EXPERTS,), 0.01)
    moe_w_gate = nrm(ks[17], (DEPTH, N_EXPERTS, D, D_EXPERT), D ** -0.5)
    moe_w_up = nrm(ks[18], (DEPTH, N_EXPERTS, D, D_EXPERT), D ** -0.5 * BETA)
    moe_w_down = nrm(ks[19], (DEPTH, N_EXPERTS, D_EXPERT, D), D_EXPERT ** -0.5 * BETA)
    ln_mix_g = 1.0 + nrm(ks[20], (DEPTH, D), 0.02)
    ln_mix_b = nrm(ks[21], (DEPTH, D), 0.02)
    ln_ffn_g = 1.0 + nrm(ks[22], (DEPTH, D), 0.02)
    ln_ffn_b = nrm(ks[23], (DEPTH, D), 0.02)
    return {'x': x, 'ret_w_in': ret_w_in, 'ret_w_out': ret_w_out, 'nsa_w_kv': nsa_w_kv,
            'cmp_pe_k': cmp_pe_k, 'cmp_pe_v': cmp_pe_v, 'cmp_k_w1': cmp_k_w1, 'cmp_k_w2': cmp_k_w2,
            'cmp_v_w1': cmp_v_w1, 'cmp_v_w2': cmp_v_w2, 'nsa_w_in': nsa_w_in, 'nsa_w_out': nsa_w_out,
            'router_w': router_w, 'router_b': router_b, 'moe_w_gate': moe_w_gate, 'moe_w_up': moe_w_up,
            'moe_w_down': moe_w_down, 'ln_mix_g': ln_mix_g, 'ln_mix_b': ln_mix_b,
            'ln_ffn_g': ln_ffn_g, 'ln_ffn_b': ln_ffn_b}


def reference(x, ret_w_in, ret_w_out, nsa_w_kv, cmp_pe_k, cmp_pe_v, cmp_k_w1, cmp_k_w2, cmp_v_w1, cmp_v_w2,
              nsa_w_in, nsa_w_out, router_w, router_b, moe_w_gate, moe_w_up, moe_w_down,
              ln_mix_g, ln_mix_b, ln_ffn_g, ln_ffn_b):
    h = x
    shared = None
    for l in range(DEPTH):
        if l < N_A_LAYERS:
            mix = retention(h, ret_w_in[l], ret_w_out[l])
        else:
            if l == N_A_LAYERS:
                shared = nsa_shared_kv(h, nsa_w_kv, cmp_pe_k, cmp_pe_v, cmp_k_w1, cmp_k_w2, cmp_v_w1, cmp_v_w2)
            j = l - N_A_LAYERS
            mix = nsa_attention(h, nsa_w_in[j], nsa_w_out[j], shared)
        h = layer_norm(ALPHA * h + mix, ln_mix_g[l], ln_mix_b[l])
        h = layer_norm(ALPHA * h + moe(h, router_w, router_b, moe_w_gate[l], moe_w_up[l], moe_w_down[l]),
                       ln_ffn_g[l], ln_ffn_b[l])
    return h
```

```python
from contextlib import ExitStack
import numpy as np
import concourse.bass as bass
import concourse.mybir as mybir
from concourse.alu_op_type import AluOpType as ALU
from concourse.bass_utils import run_bass_kernel_spmd

F32 = mybir.dt.float32
BF16 = mybir.dt.bfloat16
AF = mybir.ActivationFunctionType
AX = mybir.AxisListType

SAME_ENG_SYNC = True
STOP = ""
DBGOUT = False
_LAST = {}
GROUPS = [[0, 1, 2, 3], [4, 5, 6, 7]]


class _Op:
    __slots__ = ("eng", "fn", "deps", "dma", "semkey", "needs", "sem", "sigval", "waits", "final", "inc")

    def __init__(self, eng, fn, dma=False, semkey=None, inc=16):
        self.eng = eng
        self.fn = fn
        self.deps = set()
        self.dma = dma
        self.semkey = semkey
        self.needs = False
        self.sem = None
        self.sigval = 0
        self.waits = []
        self.final = False
        self.inc = inc


class Arena:
    def __init__(self, nc, nbytes):
        self.nc = nc
        self.beg, self.end = nc.bump_sbuf(nbytes)
        self.nbytes = nbytes
        self.n = 0

    def at(self, name, shape, dtype, off):
        sz = 1
        for d in shape[1:]:
            sz *= d
        sz *= mybir.dt.size(dtype) if hasattr(mybir.dt, "size") else {F32: 4, BF16: 2}[dtype]
        assert off + sz <= self.nbytes, (name, off, sz, self.nbytes)
        self.n += 1
        return self.nc.alloc_sbuf_tensor_at(name, shape, dtype, offset=self.beg + off)


class Prog:
    ENGS = ("pe", "act", "dve", "pool", "sp")

    def __init__(self, nc):
        self.nc = nc
        self.ops = []
        self.last_w = {}
        self.readers = {}
        self.pend_bar = {}
        self.last_eng = {}
        self.last_dma = {}
        self.slotmap = {}
        self.dynval = {}

    def barrier(self):
        allp = set(self.last_eng.values()) | set(self.last_dma.values())
        for e in self.ENGS:
            self.pend_bar[e] = set(allp) | self.pend_bar.get(e, set())

    def new_phase(self):
        self.barrier()
        self.slotmap = {}
        self.last_w = {}
        self.readers = {}

    def add(self, eng, fn, r=(), w=(), dma=False, semkey=None, inc=16):
        if dma:
            kind = "cc" if inc != 16 else eng
            if (kind, semkey) not in self.slotmap:
                n_kind = sum(1 for k in self.slotmap if k[0] == kind)
                self.slotmap[(kind, semkey)] = (kind, n_kind)
            semkey = self.slotmap[(kind, semkey)]
        op = _Op(eng, fn, dma, semkey, inc)
        idx = len(self.ops)
        deps = set()
        xr = [k for k in r if isinstance(k, tuple) and k[0] == "ps"]
        if xr:
            w = list(w) + [k for k in xr if k not in w]
        for k in r:
            lw = self.last_w.get(k)
            if lw is not None:
                deps.add(lw)
        for k in w:
            lw = self.last_w.get(k)
            if lw is not None:
                deps.add(lw)
            for rd in self.readers.get(k, ()):
                deps.add(rd)
        best = {}
        for d in deps:
            p = self.ops[d]
            if p.dma:
                d = self.last_dma[p.semkey]
                p = self.ops[d]
            if (not p.dma) and p.eng == eng and (eng == "pe" or not SAME_ENG_SYNC):
                continue
            bk = ("d", p.semkey) if p.dma else ("e", p.eng)
            if bk not in best or best[bk] < d:
                best[bk] = d
        for d in best.values():
            op.deps.add(d)
            self.ops[d].needs = True
        for d in self.pend_bar.pop(eng, ()):
            p = self.ops[d]
            if (not p.dma) and p.eng == eng:
                continue
            op.deps.add(d)
            p.needs = True
        self.ops.append(op)
        if dma:
            self.last_dma[semkey] = idx
        else:
            self.last_eng[eng] = idx
        for k in w:
            self.last_w[k] = idx
            self.readers[k] = []
        for k in r:
            if k in w:
                continue
            self.readers.setdefault(k, []).append(idx)
        return idx

    def pe(self, fn, r=(), w=()):
        return self.add("pe", fn, r, w)

    def act(self, fn, r=(), w=()):
        return self.add("act", fn, r, w)

    def dve(self, fn, r=(), w=()):
        return self.add("dve", fn, r, w)

    def pool(self, fn, r=(), w=()):
        return self.add("pool", fn, r, w)

    def dma(self, q, out, in_, r=(), w=(), semkey=None, **kw):
        assert semkey is not None
        return self.add(q, lambda e: e.dma_start(out=out, in_=in_, **kw), r, w, dma=True, semkey=semkey)

    def dma_dyn(self, q, fn, r=(), w=(), semkey=None, **kw):
        assert semkey is not None

        def f(e):
            o, i = fn(self.dynval[q])
            return e.dma_start(out=o, in_=i, **kw)
        return self.add(q, f, r, w, dma=True, semkey=semkey)

    def coll(self, in_ap, out_ap, r=(), w=(), semkey="cc"):
        return self.add("pool", lambda e: e.collective_compute(
            "AllGather", mybir.AluOpType.bypass, replica_groups=GROUPS, ins=[in_ap], outs=[out_ap]),
            r, w, dma=True, semkey=semkey, inc=1)

    def emit(self):
        nc = self.nc
        with ExitStack() as es:
            esem = {e: es.enter_context(nc.semaphore("c_" + e)) for e in ("pe", "act", "dve", "pool")}
            dsem = {}
            ecnt = {e: 0 for e in esem}
            dcnt = {}
            known = {e: {} for e in self.ENGS}
            for op in self.ops:
                if op.dma:
                    if op.semkey not in dsem:
                        dsem[op.semkey] = es.enter_context(nc.semaphore("d%d" % len(dsem)))
                        dcnt[op.semkey] = 0
                    dcnt[op.semkey] += op.inc
                    op.sem = dsem[op.semkey]
                    op.sigval = dcnt[op.semkey]
                elif op.needs:
                    ecnt[op.eng] += 1
                    op.sem = esem[op.eng]
                    op.sigval = ecnt[op.eng]
                need = {}
                for d in op.deps:
                    p = self.ops[d]
                    key = id(p.sem)
                    if key not in need or need[key][1] < p.sigval:
                        need[key] = (p.sem, p.sigval)
                kn = known[op.eng]
                for key, (sem, val) in need.items():
                    if kn.get(key, 0) >= val:
                        continue
                    kn[key] = val
                    op.waits.append((sem, val))
            print("[prog] ops=%d sems=%d eng_counts=%s max_dma_cnt=%d" % (
                len(self.ops), len(dsem) + 4, ecnt, max(dcnt.values()) if dcnt else 0), flush=True)
            finals = [(dsem[k], dcnt[k]) for k in dsem]
            byeng = {e: [op for op in self.ops if op.eng == e] for e in self.ENGS}
            with nc.Block() as block:
                def run(eng, name, is_last):
                    if name == "sp":
                        self.dynval[name] = nc.sync.partition_id() % 4
                    elif name == "pool":
                        self.dynval[name] = nc.gpsimd.partition_id() % 4
                    for op in byeng[name]:
                        for sem, val in op.waits:
                            eng.wait_ge(sem, val)
                        inst = op.fn(eng)
                        if op.dma:
                            inst.then_inc(op.sem, op.inc)
                        elif op.needs:
                            inst.then_inc(op.sem, 1)
                    if is_last:
                        for sem, val in finals:
                            eng.wait_ge(sem, val)

                @block.tensor
                def _(e):
                    run(e, "pe", False)

                @block.scalar
                def _(e):
                    run(e, "act", False)

                @block.vector
                def _(e):
                    run(e, "dve", False)

                @block.gpsimd
                def _(e):
                    run(e, "pool", False)

                @block.sync
                def _(e):
                    run(e, "sp", True)


ALPHA = 4.0 ** 0.25
LN_EPS = 1e-5
NT = 2048
NTT = NT // 128
NE = 16
S = 8192
NCH = 64
NEGB = -30000.0
NQB = 64
TYCOL = [0, 256, 512, 1024, 1536, 1792, 2048, 2304]


def build_fused():
    nc = bass.Bass("TRN2", target_bir_lowering=False)
    din = lambda n, s: nc.dram_tensor(n, s, F32, kind="ExternalInput").ap()
    xT = din("xT", [1024, S]); wr = din("wr", [1024, 1536])
    cosT = din("cosT", [128, S]); sinT = din("sinT", [128, S])
    dm_d = din("dm", [128, 128]); qd_d = din("qd", [128, 128]); kd_d = din("kd", [128, 1]); cd_d = din("cd", [128, 1])
    ident_d = din("ident", [128, 128]); sel_d = din("sel", [16, NE * 128])
    xres = din("xres", [NT, 1024])
    wout0 = din("wout0", [2048, 1024]); wout1 = din("wout1", [1024, 1024])
    lnp = din("lnp", [8, 1024])
    rw = din("rw", [1024, 16]); rbias = din("rbias", [1, 16])
    wg = din("wg", [2, NE, 1024, 512]); wu = din("wu", [2, NE, 1024, 512]); wd = din("wd", [2, NE, 512, 1024])
    wp = din("wp", [1024, 2608]); cs_d = din("cs", [NT, 64])
    w1k = din("w1k", [2048, 256]); w2k = din("w2k", [256, 64]); w1v = din("w1v", [2048, 256]); w2v = din("w2v", [256, 64])
    pek = din("pek", [128, 16]); pev = din("pev", [128, 16])
    ind_d = din("ind", [64, 32 * 128])
    caus_d = din("caus4", [128, 512]); anti_d = din("anti4", [128, 512])
    cmk_d = din("cmk", [128, 16 * 2 * 128]); ov_d = din("ov", [512, 128]); bt_d = din("bt", [128, 256])
    hout = nc.dram_tensor("hout", [NT, 1024], F32, kind="ExternalOutput").ap()
    idram = lambda n, s, d: nc.dram_tensor(n, s, d)
    e1_in = [idram("e1i%d" % c, [512, 1024], BF16) for c in range(8)]
    e1_out = [idram("e1o%d" % c, [2048, 1024], BF16) for c in range(8)]
    e2x_in = [[idram("e2xi%d_%d" % (ty, th), [256, 1024], BF16) for th in range(2)] for ty in range(8)]
    e2x_out = [[idram("e2xo%d_%d" % (ty, th), [1024, 1024], BF16) for th in range(2)] for ty in range(8)]
    e2v_in = [idram("e2vi%d" % v, [512, 512], BF16) for v in range(4)]
    e2v_out = [idram("e2vo%d" % v, [2048, 512], BF16) for v in range(4)]
    e2g_in = idram("e2gi", [48, NT], F32)
    e2g_out = idram("e2go", [192, NT], F32)
    e3_in = [idram("e3i%d" % c, [256, 512], BF16) for c in range(16)]
    e3_out = [idram("e3o%d" % c, [1024, 512], BF16) for c in range(16)]
    h1d = nc.dram_tensor("h1d", [NT, 1024], F32, kind="ExternalOutput") if DBGOUT else idram("h1d", [NT, 1024], F32)
    xkind = dict(kind="ExternalOutput") if DBGOUT else {}
    locx = [[nc.dram_tensor("lx%d_%d" % (ty, th), [64, 4096], BF16, **xkind) for th in range(2)] for ty in range(8)]
    locg = nc.dram_tensor("lgt", [48, NT], F32, **xkind)
    dbg3 = nc.dram_tensor("dbg3", [16, 256, 512], BF16, kind="ExternalOutput") if DBGOUT else None
    dbg4 = nc.dram_tensor("dbg4", [2, 1024, 512], BF16, kind="ExternalOutput") if DBGOUT else None
    ds = bass.ds

    with ExitStack() as es:
        AR = Arena(nc, 200 * 1024)
        PS = [es.enter_context(nc.psum_tensor("ps%d" % i, [128, 512], F32)) for i in range(8)]
        P = Prog(nc)
        cur = [0]
        pfx = [""]

        def sb(n, s, d, off=None):
            sz = 1
            for x in s[1:]:
                sz *= x
            sz *= (4 if d == F32 else 2)
            sz = (sz + 31) // 32 * 32
            if off is None:
                off = cur[0]
                cur[0] += sz
            return AR.at(pfx[0] + n, s, d, off)

        def phase_ret():
            pfx[0] = "A_"; cur[0] = 0
            W = sb("W", [128, 8, 1536], BF16)
            CT = sb("CT", [128, S], F32); ST = sb("ST", [128, S], F32)
            DM = sb("DM", [128, 128], F32); QD = sb("QD", [128, 128], F32); KDv = sb("KDv", [128, 1], F32); CDv = sb("CDv", [128, 1], F32)
            IDB = sb("IDB", [128, 128], BF16)
            S32 = sb("S32", [128, 2, 512], F32); Sb = sb("Sb", [128, 2, 512], BF16)
            XT = [sb("XT%d" % i, [128, 8, 128], BF16) for i in range(2)]
            T = [sb("T%d" % i, [128, 128], F32) for i in range(4)]
            QT = [sb("QT%d" % i, [128, 2, 128], BF16) for i in range(2)]; KT = [sb("KT%d" % i, [128, 2, 128], BF16) for i in range(2)]
            QDT = [sb("QDT%d" % i, [128, 2, 128], BF16) for i in range(2)]
            V = [sb("V%d" % i, [128, 512], BF16) for i in range(2)]; SGt = [sb("SGt%d" % i, [128, 512], F32) for i in range(2)]
            INT = sb("INT", [128, 128], BF16); KD = sb("KD", [128, 2, 128], BF16)
            ON = sb("ON", [128, 512], F32); YB = [sb("YB%d" % i, [128, 512], BF16) for i in range(2)]
            YTT = [sb("YTT%d" % i, [128, 4, 128], BF16) for i in range(2)]
            st6 = sb("st6", [128, 6], F32); mv = sb("mv", [128, 2], F32); rstd = sb("rstd", [128, 1], F32)
            PSt = PS[7][:].bitcast(BF16)

            P.dma("pool", W[:], wr.rearrange("(k p) n -> p k n", p=128), w=["W"], semkey="W")
            for q4 in range(4):
                P.dma("sp", CT[:, q4 * 2048:(q4 + 1) * 2048], cosT[:, q4 * 2048:(q4 + 1) * 2048], w=[("CT", q4)], semkey=("CT", q4))
                P.dma("sp", ST[:, q4 * 2048:(q4 + 1) * 2048], sinT[:, q4 * 2048:(q4 + 1) * 2048], w=[("ST", q4)], semkey=("ST", q4))
            P.dma("sp", DM[:], dm_d, w=["DM"], semkey="DM")
            P.dma("sp", QD[:], qd_d, w=["QD"], semkey="QD")
            P.dma("sp", KDv[:], kd_d, w=["KDv"], semkey="KDv")
            P.dma("sp", CDv[:], cd_d, w=["CDv"], semkey="CDv")
            P.dma("pool", IDB[:], ident_d, w=["IDB"], semkey="IDB")
            P.dve(lambda e: e.memset(S32[:], 0.0), w=["S32"])
            P.dve(lambda e: e.memset(Sb[:], 0.0), w=["Sb"])
            xTv = xT.rearrange("(k p) t -> p k t", p=128)

            def f_load(n):
                xb = n % 2
                P.dma("pool", XT[xb][:], xTv[:, :, n * 128:(n + 1) * 128], w=[("XT", xb)], semkey=("XT", xb))

            def f_mm(n, gi):
                xb = n % 2
                X = XT[xb]
                if gi < 4:
                    reg = gi
                    for kc in range(8):
                        P.pe(lambda e, reg=reg, kc=kc, X=X: e.matmul(PS[0][:, reg * 128:(reg + 1) * 128], W[:, kc, reg * 128:(reg + 1) * 128],
                                                                      X[:, kc, :], start=(kc == 0), stop=(kc == 7)),
                             r=["W", ("XT", xb)], w=[("ps", 0)])
                else:
                    bank = 1 if gi == 4 else 2
                    c0 = 512 if gi == 4 else 1024
                    for kc in range(8):
                        P.pe(lambda e, kc=kc, X=X, bank=bank, c0=c0: e.matmul(PS[bank][:], X[:, kc, :], W[:, kc, c0:c0 + 512], start=(kc == 0), stop=(kc == 7)),
                             r=["W", ("XT", xb)], w=[("ps", bank)])

            def f_post(n):
                b_ = n % 2
                tsl = slice(n * 128, (n + 1) * 128)
                q4 = n // 16
                C = CT[:, tsl]; Sn = ST[:, tsl]
                for which, dst in ((0, QT[b_]), (1, KT[b_])):
                    A = PS[0][:, (2 * which) * 128:(2 * which + 1) * 128]
                    B = PS[0][:, (2 * which + 1) * 128:(2 * which + 2) * 128]
                    dk = ("QT", b_) if which == 0 else ("KT", b_)
                    P.dve(lambda e, A=A, C=C: e.tensor_tensor(T[0][:], A, C, ALU.mult), r=[("ps", 0), ("CT", q4)], w=["T0"])
                    P.dve(lambda e, B=B, Sn=Sn: e.tensor_tensor(T[1][:], B, Sn, ALU.mult), r=[("ps", 0), ("ST", q4)], w=["T1"])
                    P.dve(lambda e, B=B, C=C: e.tensor_tensor(T[2][:], B, C, ALU.mult), r=[("ps", 0), ("CT", q4)], w=["T2"])
                    P.dve(lambda e, A=A, Sn=Sn: e.tensor_tensor(T[3][:], A, Sn, ALU.mult), r=[("ps", 0), ("ST", q4)], w=["T3"])
                    P.dve(lambda e, dst=dst: e.tensor_tensor(dst[:, 0, :], T[0][:], T[1][:], ALU.subtract), r=["T0", "T1"], w=[dk])
                    P.dve(lambda e, dst=dst: e.tensor_tensor(dst[:, 1, :], T[2][:], T[3][:], ALU.add), r=["T2", "T3"], w=[dk])
                P.dve(lambda e: e.tensor_tensor(QDT[b_][:], QT[b_][:], QD[:].unsqueeze(1).broadcast_to([128, 2, 128]), ALU.mult),
                      r=[("QT", b_), "QD"], w=[("QDT", b_)])
                P.act(lambda e: e.copy(V[b_][:], PS[1][:]), r=[("ps", 1)], w=[("V", b_)])
                P.act(lambda e: e.activation(out=SGt[b_][:], in_=PS[2][:], func=AF.Silu), r=[("ps", 2)], w=[("SGt", b_)])

            def b_s0(n):
                b_ = n % 2
                for dc in range(2):
                    P.pe(lambda e, dc=dc: e.matmul(PS[3][:, 0:128], KT[b_][:, dc, :], QT[b_][:, dc, :], start=(dc == 0), stop=(dc == 1)),
                         r=[("KT", b_), ("QT", b_)], w=[("ps", 3)])
                P.dve(lambda e: e.tensor_tensor(INT[:], PS[3][:, 0:128], DM[:], ALU.mult), r=[("ps", 3), "DM"], w=["INT"])
                for dc in range(2):
                    P.pe(lambda e, dc=dc: e.transpose(PSt[:, dc * 128:(dc + 1) * 128], KT[b_][:, dc, :], IDB[:]), r=[("KT", b_), "IDB"], w=[("ps", 7)])
                P.dve(lambda e: e.tensor_scalar(KD[:], PSt[:, 0:256].rearrange("p (a b) -> p a b", a=2), KDv[:, 0:1], None, ALU.mult),
                      r=[("ps", 7), "KDv"], w=["KD"])

            def b_s1(n):
                b_ = n % 2
                P.pe(lambda e: e.matmul(PS[4][:], INT[:], V[b_][:], start=True, stop=False), r=["INT", ("V", b_)], w=[("ps", 4)])
                for dc in range(2):
                    P.pe(lambda e, dc=dc: e.matmul(PS[4][:], QDT[b_][:, dc, :], Sb[:, dc, :], start=False, stop=(dc == 1)),
                         r=[("QDT", b_), "Sb"], w=[("ps", 4)])
                for dc in range(2):
                    P.pe(lambda e, dc=dc: e.matmul(PS[5 + dc][:], KD[:, dc, :], V[b_][:], start=True, stop=True), r=["KD", ("V", b_)], w=[("ps", 5 + dc)])
                    P.dve(lambda e, dc=dc: e.scalar_tensor_tensor(S32[:, dc, :], S32[:, dc, :], CDv[:, 0:1], PS[5 + dc][:], ALU.mult, ALU.add),
                          r=[("ps", 5 + dc), "S32", "CDv"], w=["S32"])
                P.act(lambda e: e.copy(Sb[:], S32[:]), r=["S32"], w=["Sb"])

            def b_s2(n):
                b_ = n % 2
                P.dve(lambda e: e.bn_stats(st6[:], PS[4][:]), r=[("ps", 4)], w=["st6"])
                P.dve(lambda e: e.bn_aggr(mv[:], st6[:]), r=["st6"], w=["mv"])
                P.dve(lambda e: e.tensor_scalar(rstd[:], mv[:, 1:2], 1e-5, None, ALU.add), r=["mv", "rstd"], w=["rstd"])
                P.act(lambda e: e.activation(out=rstd[:], in_=rstd[:], func=AF.Sqrt), r=["rstd"], w=["rstd"])
                P.dve(lambda e: e.reciprocal(rstd[:], rstd[:]), r=["rstd"], w=["rstd"])
                P.dve(lambda e: e.tensor_scalar(ON[:], PS[4][:], mv[:, 0:1], rstd[:, 0:1], ALU.subtract, ALU.mult),
                      r=[("ps", 4), "mv", "rstd"], w=["ON"])
                yb = YB[n % 2]
                P.dve(lambda e, yb=yb: e.tensor_tensor(yb[:], ON[:], SGt[b_][:], ALU.mult), r=["ON", ("SGt", b_)], w=[("YB", n % 2)])

            def b_s3(n):
                yb = YB[n % 2]; ytt = YTT[n % 2]
                for j in range(4):
                    P.pe(lambda e, j=j, yb=yb: e.transpose(PSt[:, 512 + j * 128:512 + (j + 1) * 128], yb[:, j * 128:(j + 1) * 128], IDB[:]),
                         r=[("YB", n % 2), "IDB"], w=[("ps", 7)])
                P.act(lambda e, ytt=ytt: e.copy(ytt[:], PSt[:, 512:1024].rearrange("p (j t) -> p j t", j=4)), r=[("ps", 7)], w=[("YTT", n % 2)])
                c = n % 16; qq = n // 16
                c0_ = qq * 256 + (c % 2) * 128
                P.dma("sp", e1_in[c // 2].ap().rearrange("(j p) t -> p j t", p=128)[:, :, c0_:c0_ + 128], ytt[:],
                      r=[("YTT", n % 2)], w=[("e1i", n)], semkey=("YTT", n % 2))

            f_load(0)
            for gi in range(6):
                f_mm(0, gi)
            f_post(0)
            for n in range(NCH):
                nx = n + 1 < NCH
                if nx:
                    f_load(n + 1)
                b_s0(n)
                if nx:
                    f_mm(n + 1, 0); f_mm(n + 1, 1)
                b_s1(n)
                if nx:
                    f_mm(n + 1, 2)
                b_s2(n)
                if nx:
                    f_mm(n + 1, 3); f_mm(n + 1, 4)
                b_s3(n)
                if nx:
                    f_mm(n + 1, 5)
                    f_post(n + 1)
            for c in range(8):
                P.coll(e1_in[c].ap().opt(), e1_out[c].ap().opt(), r=[("e1i", n_) for n_ in range(NCH) if (n_ % 16) // 2 == c],
                       w=[("e1o", c)], semkey="cc")

        def phase_post(L, KM, with_proj, a_out, a_key, hres, hdst, wout):
            pfx[0] = "B%d_" % L; cur[0] = 0
            ln1g = lnp[4 * L + 0:4 * L + 1, :]; ln1b = lnp[4 * L + 1:4 * L + 2, :]
            ln2g = lnp[4 * L + 2:4 * L + 3, :]; ln2b = lnp[4 * L + 3:4 * L + 4, :]
            Y = sb("Y", [128, NTT, 1024], F32)
            HTb = sb("HTb", [128, 8, NT], BF16)
            BIGW = sb("BIGW", [128, 6 * 4096], BF16)
            LG = sb("LG", [128, 1024], F32); LB = sb("LB", [128, 1024], F32)
            RW = sb("RW", [128, 8, 16], F32); RB = sb("RB", [128, 16], F32)
            IDN = sb("IDN", [128, 128], F32)
            SEL = sb("SEL", [16, NE * 128], BF16)
            WT = sb("WT", [16, NT], BF16)
            WPAD = sb("WPAD", [128, 128], F32)
            st6 = sb("st6", [128, 2, 6], F32); mv = sb("mv", [128, 2], F32); rstd = sb("rstd", [128, 1], F32)
            R = {n: sb("r_" + n, [128, 16], F32) for n in ("aff", "ss", "eq", "a2", "ms", "eq1", "ms2", "ch", "w")}
            R4 = {n: sb("r4_" + n, [128, 4], F32) for n in ("m1", "m2", "gs", "ing")}
            R1 = {n: sb("r1_" + n, [128, 1], F32) for n in ("gm", "t1", "t2", "ws", "rws")}
            scr0 = cur[0]
            Asb = [sb("Asb%d" % i, [128, KM, 128], BF16) for i in range(2)]
            HR = [sb("HR%d" % i, [128, 1024], F32) for i in range(2)]
            U = sb("U", [128, 1024], F32)
            HA = sb("HA", [128, 1024], F32)
            HT32 = [sb("HT32_%d" % i, [128, 8, 128], F32) for i in range(2)]
            cur[0] = scr0
            WBC = [sb("WBC%d" % i, [128, 512], BF16) for i in range(2)]
            SG = [sb("SG%d" % i, [128, 512], BF16) for i in range(2)]
            TU = [sb("TU%d" % i, [128, 512], BF16) for i in range(2)]
            ACTV = [sb("ACTV%d" % i, [128, 4, 512], BF16) for i in range(2)]
            if with_proj:
                cur[0] = scr0
                CS = [sb("CS%d" % i, [128, 64], F32) for i in range(2)]
                HB16 = [sb("HB16_%d" % i, [128, 8, 128], BF16) for i in range(2)]
                PR = [sb("PR%d" % i, [128, 2608], F32) for i in range(2)]
                T1 = sb("T1", [128, 256], F32); T2 = sb("T2", [128, 256], F32)
                XTt = [sb("XTt%d" % i, [128, 16, 128], BF16) for i in range(2)]
                VB = [sb("VB%d" % i, [128, 512], BF16) for i in range(1)]
                GTt = [sb("GTt%d" % i, [48, 128], F32) for i in range(1)]

            Wout = BIGW[:, 0:KM * 1024].rearrange("p (k n) -> p k n", k=KM)
            ring = [BIGW[:, s * 4096:(s + 1) * 4096] for s in range(6)]

            P.dve(lambda e: e.memset(WPAD[:], 0.0), w=["w"])
            P.dma("sp", IDN[:], ident_d, w=["IDN"], semkey="IDN")
            P.dma("sp", RW[:], rw.rearrange("(k p) n -> p k n", p=128), w=["RW"], semkey="RW")
            P.dma("sp", RB[:], rbias.partition_broadcast(128), w=["RB"], semkey="RB")
            P.dma("sp", LG[:], ln1g.partition_broadcast(128), w=["LG"], semkey="LG")
            P.dma("sp", LB[:], ln1b.partition_broadcast(128), w=["LB"], semkey="LB")
            P.dma("pool", SEL[:], sel_d, w=["SEL"], semkey="SEL")
            woutv = wout.rearrange("(k p) n -> p k n", p=128)
            for k0 in range(0, KM, 4):
                P.dma("pool", Wout[:, k0:k0 + 4, :], woutv[:, k0:k0 + 4, :], w=[("Wout", k0)], semkey=("Wout", k0))
            WoutKeys = [("Wout", k0) for k0 in range(0, KM, 4)]

            def layer_norm(src_key, src, dst, dst_keys, extra_r=()):
                P.dve(lambda e: e.bn_stats(st6[:, 0, :], src[:, 0:512]), r=[src_key], w=["st6a"])
                P.dve(lambda e: e.bn_stats(st6[:, 1, :], src[:, 512:1024]), r=[src_key], w=["st6b"])
                P.dve(lambda e: e.bn_aggr(mv[:], st6[:].rearrange("p a b -> p (a b)")), r=["st6a", "st6b"], w=["mv"])
                P.dve(lambda e: e.tensor_scalar(rstd[:], mv[:, 1:2], LN_EPS, None, ALU.add), r=["mv", "rstd"], w=["rstd"])
                P.act(lambda e: e.activation(out=rstd[:], in_=rstd[:], func=AF.Sqrt), r=["rstd"], w=["rstd"])
                P.dve(lambda e: e.reciprocal(rstd[:], rstd[:]), r=["rstd"], w=["rstd"])
                P.dve(lambda e: e.tensor_scalar(dst, src, mv[:, 0:1], rstd[:, 0:1], ALU.subtract, ALU.mult),
                      r=[src_key, "mv", "rstd"] + list(extra_r), w=list(dst_keys))
                P.dve(lambda e: e.tensor_tensor(dst, dst, LG[:], ALU.mult), r=list(dst_keys) + ["LG"], w=list(dst_keys))
                P.dve(lambda e: e.tensor_tensor(dst, dst, LB[:], ALU.add), r=list(dst_keys) + ["LB"], w=list(dst_keys))

            def s1_a1(tt):
                ab = tt % 2
                A = Asb[ab]; H = HR[ab]
                P.dma_dyn("sp" if L == 0 else "pool", lambda g, A=A, tt=tt: (A[:], a_out(tt, g)),
                          w=[("A", ab)], semkey=("A", ab))
                P.dma("sp", H[:], hres[tt * 128:(tt + 1) * 128, :], w=[("HR", ab)], semkey=("HR", ab))
                for half in range(2):
                    ps = PS[half]
                    for k in range(KM):
                        P.pe(lambda e, ps=ps, k=k, half=half, A=A: e.matmul(
                            ps[:], A[:, k, :], Wout[:, k, half * 512:(half + 1) * 512], start=(k == 0), stop=(k == KM - 1)),
                            r=[("A", ab), ("Wout", (k // 4) * 4)], w=[("ps", half)])
                    P.dve(lambda e, ps=ps, half=half, H=H: e.scalar_tensor_tensor(
                        U[:, half * 512:(half + 1) * 512], H[:, half * 512:(half + 1) * 512], ALPHA, ps[:], ALU.mult, ALU.add),
                        r=[("ps", half), ("HR", ab)], w=["U"])
                layer_norm("U", U[:], HA[:], ["hA"])
                P.act(lambda e, tt=tt: e.mul(Y[:, tt, :], HA[:], ALPHA), r=["hA"], w=[("Y", tt)])
            def s1_a2(tt):
                ab = tt % 2
                for kc in range(8):
                    pb = 2 + (kc % 2)
                    P.pe(lambda e, kc=kc, pb=pb: e.transpose(PS[pb][:, 0:128], HA[:, kc * 128:(kc + 1) * 128], IDN[:]),
                         r=["hA", "IDN"], w=[("ps", pb)])
                    P.act(lambda e, kc=kc, pb=pb, tt=tt: e.copy(HTb[:, kc, tt * 128:(tt + 1) * 128], PS[pb][:, 0:128]),
                          r=[("ps", pb)], w=[("HTb", tt // 4, kc)])
                    P.dve(lambda e, kc=kc, pb=pb: e.tensor_copy(HT32[tt % 2][:, kc, :], PS[pb][:, 0:128]),
                          r=[("ps", pb)], w=[("HT32", tt % 2, kc)])
            def s1_bm(tt):
                ab = tt % 2
                for kc in range(8):
                    P.pe(lambda e, kc=kc: e.matmul(PS[4][:, 0:16], HT32[tt % 2][:, kc, :], RW[:, kc, :], start=(kc == 0), stop=(kc == 7)),
                         r=[("HT32", tt % 2, kc), "RW"], w=[("ps", 4)])
                r = R; r4 = R4; r1 = R1
                P.act(lambda e: e.activation(out=r["aff"][:], in_=PS[4][:, 0:16], func=AF.Sigmoid), r=[("ps", 4)], w=["aff"])
                P.dve(lambda e: e.tensor_tensor(r["ss"][:], r["aff"][:], RB[:], ALU.add), r=["aff", "RB"], w=["ss"])
                ss3 = r["ss"][:].rearrange("p (g k) -> p g k", g=4)
                P.dve(lambda e: e.tensor_reduce(r4["m1"][:], ss3, AX.X, ALU.max), r=["ss"], w=["m1"])
                P.dve(lambda e: e.tensor_tensor(r["eq"][:].rearrange("p (g k) -> p g k", g=4), ss3,
                                                 r4["m1"][:].unsqueeze(2).broadcast_to([128, 4, 4]), ALU.is_equal),
                      r=["ss", "m1"], w=["eq"])
                P.dve(lambda e: e.scalar_tensor_tensor(r["a2"][:], r["eq"][:], -1.0e9, r["ss"][:], ALU.mult, ALU.add),
                      r=["eq", "ss"], w=["a2"])
                P.dve(lambda e: e.tensor_reduce(r4["m2"][:], r["a2"][:].rearrange("p (g k) -> p g k", g=4), AX.X, ALU.max),
                      r=["a2"], w=["m2"])
                P.dve(lambda e: e.tensor_tensor(r4["gs"][:], r4["m1"][:], r4["m2"][:], ALU.add), r=["m1", "m2"], w=["gs"])
                P.dve(lambda e: e.tensor_reduce(r1["gm"][:], r4["gs"][:], AX.X, ALU.max), r=["gs"], w=["gm"])
                P.dve(lambda e: e.tensor_scalar(r4["ing"][:], r4["gs"][:], r1["gm"][:, 0:1], None, ALU.is_ge), r=["gs", "gm"], w=["ing"])
                P.dve(lambda e: e.tensor_scalar(r4["ing"][:], r4["ing"][:], 1.0, 1.0e9, ALU.subtract, ALU.mult), r=["ing"], w=["ing"])
                P.dve(lambda e: e.tensor_tensor(r["ms"][:].rearrange("p (g k) -> p g k", g=4), ss3,
                                                 r4["ing"][:].unsqueeze(2).broadcast_to([128, 4, 4]), ALU.add),
                      r=["ss", "ing"], w=["ms"])
                P.dve(lambda e: e.tensor_reduce(r1["t1"][:], r["ms"][:], AX.X, ALU.max), r=["ms"], w=["t1"])
                P.dve(lambda e: e.tensor_scalar(r["eq1"][:], r["ms"][:], r1["t1"][:, 0:1], -1.0e9, ALU.is_equal, ALU.mult),
                      r=["ms", "t1"], w=["eq1"])
                P.dve(lambda e: e.tensor_tensor(r["ms2"][:], r["eq1"][:], r["ms"][:], ALU.add), r=["eq1", "ms"], w=["ms2"])
                P.dve(lambda e: e.tensor_reduce(r1["t2"][:], r["ms2"][:], AX.X, ALU.max), r=["ms2"], w=["t2"])
                P.dve(lambda e: e.tensor_scalar(r["ch"][:], r["ms"][:], r1["t2"][:, 0:1], None, ALU.is_ge), r=["ms", "t2"], w=["ch"])
                P.dve(lambda e: e.tensor_tensor(WPAD[:, 0:16], r["ch"][:], r["aff"][:], ALU.mult), r=["ch", "aff"], w=["w"])
                P.dve(lambda e: e.tensor_reduce(r1["ws"][:], WPAD[:, 0:16], AX.X, ALU.add), r=["w"], w=["ws"])
                P.dve(lambda e: e.reciprocal(r1["rws"][:], r1["ws"][:]), r=["ws"], w=["rws"])
                P.dve(lambda e: e.tensor_scalar(WPAD[:, 0:16], WPAD[:, 0:16], r1["rws"][:, 0:1], None, ALU.mult), r=["w", "rws"], w=["w"])
            def s1_bt(tt):
                P.pe(lambda e: e.transpose(PS[5][:, 0:128], WPAD[:], IDN[:]), r=["w", "IDN"], w=[("ps", 5)])
                P.act(lambda e, tt=tt: e.copy(WT[:, tt * 128:(tt + 1) * 128], PS[5][0:16, 0:128]), r=[("ps", 5)], w=[("WT", tt // 4)])
            s1_a1(0)
            s1_a2(0)
            for tt in range(NTT):
                if tt + 1 < NTT:
                    s1_a1(tt + 1)
                s1_bm(tt)
                if tt + 1 < NTT:
                    s1_a2(tt + 1)
                s1_bt(tt)

            P.barrier()
            wgv = wg[L].rearrange("e (k p) n -> e p k n", p=128)
            wuv = wu[L].rearrange("e (k p) n -> e p k n", p=128)
            wdv = wd[L].rearrange("e (k p) n -> e p k n", p=128)
            nload = [0]

            def ring_load(src, shape_k):
                s = nload[0] % 6
                first = nload[0] < 6
                nload[0] += 1
                dst = ring[s].rearrange("p (k n) -> p k n", k=shape_k)
                wk = [("ring", s)] + (WoutKeys if first else [])
                P.dma("pool", dst, src, w=wk, semkey=("ring", s))
                return s, dst

            pend = []

            def prefetch(e):
                pend.append((ring_load(wgv[e], 8), ring_load(wuv[e], 8), ring_load(wdv[e], 4)))
            prefetch(0)
            it = 0
            for e_ in range(NE):
                if e_ + 1 < NE:
                    prefetch(e_ + 1)
                (sg_, G), (su_, Uw), (sd_, Dn) = pend.pop(0)
                for tg in range(4):
                    wb = WBC[it % 2]
                    P.pe(lambda e, e_=e_, tg=tg: e.matmul(PS[6][:], SEL[:, e_ * 128:(e_ + 1) * 128], WT[:, tg * 512:(tg + 1) * 512],
                                                           start=True, stop=True), r=["SEL", ("WT", tg)], w=[("ps", 6)])
                    P.act(lambda e, wb=wb: e.copy(wb[:], PS[6][:]), r=[("ps", 6)], w=[("WBC", it % 2)])
                    AV = ACTV[it % 2]
                    for hc in range(4):
                        pg = PS[0 + (hc % 2)]; pu = PS[2 + (hc % 2)]
                        for kc in range(8):
                            P.pe(lambda e, pg=pg, kc=kc, hc=hc, tg=tg, G=G: e.matmul(
                                pg[:], G[:, kc, hc * 128:(hc + 1) * 128], HTb[:, kc, tg * 512:(tg + 1) * 512],
                                start=(kc == 0), stop=(kc == 7)), r=[("ring", sg_), ("HTb", tg, kc)], w=[("ps", 0 + hc % 2)])
                        for kc in range(8):
                            P.pe(lambda e, pu=pu, kc=kc, hc=hc, tg=tg, Uw=Uw: e.matmul(
                                pu[:], Uw[:, kc, hc * 128:(hc + 1) * 128], HTb[:, kc, tg * 512:(tg + 1) * 512],
                                start=(kc == 0), stop=(kc == 7)), r=[("ring", su_), ("HTb", tg, kc)], w=[("ps", 2 + hc % 2)])
                        sgt = SG[hc % 2]; tut = TU[hc % 2]
                        P.act(lambda e, sgt=sgt, pg=pg: e.activation(out=sgt[:], in_=pg[:], func=AF.Silu),
                              r=[("ps", 0 + hc % 2)], w=[("SG", hc % 2)])
                        P.dve(lambda e, tut=tut, pu=pu, sgt=sgt: e.tensor_tensor(tut[:], pu[:], sgt[:], ALU.mult),
                              r=[("ps", 2 + hc % 2), ("SG", hc % 2)], w=[("TU", hc % 2)])
                        P.dve(lambda e, AV=AV, hc=hc, tut=tut, wb=wb: e.tensor_tensor(AV[:, hc, :], tut[:], wb[:], ALU.mult),
                              r=[("TU", hc % 2), ("WBC", it % 2)], w=[("AV", it % 2, hc)])
                    for tl in range(4):
                        tt = tg * 4 + tl
                        for half in range(2):
                            py = PS[4 + half]
                            for hc in range(4):
                                P.pe(lambda e, py=py, AV=AV, hc=hc, tl=tl, half=half, Dn=Dn: e.matmul(
                                    py[:], AV[:, hc, tl * 128:(tl + 1) * 128], Dn[:, hc, half * 512:(half + 1) * 512],
                                    start=(hc == 0), stop=(hc == 3)), r=[("AV", it % 2, hc), ("ring", sd_)], w=[("ps", 4 + half)])
                            P.dve(lambda e, py=py, tt=tt, half=half: e.tensor_tensor(
                                Y[:, tt, half * 512:(half + 1) * 512], Y[:, tt, half * 512:(half + 1) * 512], py[:], ALU.add),
                                r=[("ps", 4 + half), ("Y", tt)], w=[("Y", tt)])
                    it += 1

            P.barrier()
            P.dma("sp", LG[:], ln2g.partition_broadcast(128), w=["LG"], semkey="LG")
            P.dma("sp", LB[:], ln2b.partition_broadcast(128), w=["LB"], semkey="LB")
            if with_proj:
                WKV = BIGW[:, 0:8 * 1536].rearrange("p (k n) -> p k n", k=8)
                WIN = HTb[:].rearrange("p k t -> p (k t)")[:, 0:8 * 1072].rearrange("p (k n) -> p k n", k=8)
                allring = [("ring", s) for s in range(6)] + WoutKeys
                wpv = wp.rearrange("(k p) n -> p k n", p=128)
                P.dma("pool", WKV, wpv[:, :, 0:1536], w=allring + ["WKV"], semkey="WKV")
                allht = [("HTb", tg, kc) for tg in range(4) for kc in range(8)]
                P.dma("pool", WIN, wpv[:, :, 1536:2608], w=allht + ["WIN"], semkey="WIN")
            def s3_ln(tt):
                ob = tt % 2
                layer_norm(("Y", tt), Y[:, tt, :], Y[:, tt, :], [("Y", tt)])
                P.dma("sp", hdst[tt * 128:(tt + 1) * 128, :], Y[:, tt, :], r=[("Y", tt)], w=[("hdst", tt)], semkey=("hout", tt % 4))
                if with_proj:
                    P.dma("sp", CS[ob][:], cs_d[tt * 128:(tt + 1) * 128, :], w=[("CS", ob)], semkey=("CS", ob))

            def s3_tr(tt):
                ob = tt % 2
                hb = HB16[ob]
                for kc in range(8):
                    pb = 6 + (kc % 2)
                    P.pe(lambda e, kc=kc, pb=pb, tt=tt: e.transpose(PS[pb][:, 0:128], Y[:, tt, kc * 128:(kc + 1) * 128], IDN[:]),
                         r=[("Y", tt), "IDN"], w=[("ps", pb)])
                    P.act(lambda e, kc=kc, pb=pb, hb=hb: e.copy(hb[:, kc, :], PS[pb][:, 0:128]), r=[("ps", pb)], w=[("HB16", ob, kc)])

            PRK = {}

            def s3_grp(tt):
                ob = tt % 2
                hb = HB16[ob]; cs = CS[ob]; pr = PR[ob]
                groups = [(WKV, 0, 512, 0, "WKV"), (WKV, 512, 512, 512, "WKV"), (WKV, 1024, 512, 1024, "WKV"),
                          (WIN, 0, 512, 1536, "WIN"), (WIN, 512, 512, 2048, "WIN"), (WIN, 1024, 48, 2560, "WIN")]
                prk = []
                for gi, (Wv, c0, ncol, oc, wkey) in enumerate(groups):
                    pb = gi % 6
                    ps = PS[pb]
                    for kc in range(8):
                        P.pe(lambda e, ps=ps, kc=kc, Wv=Wv, c0=c0, ncol=ncol, hb=hb: e.matmul(
                            ps[:, 0:ncol], hb[:, kc, :], Wv[:, kc, c0:c0 + ncol], start=(kc == 0), stop=(kc == 7)),
                            r=[("HB16", ob, kc), wkey], w=[("ps", pb)])
                    if gi == 5:
                        P.act(lambda e, ps=ps, pr=pr: e.activation(out=pr[:, 2560:2608], in_=ps[:, 0:48], func=AF.Sigmoid),
                              r=[("ps", pb)], w=[("PR", ob, gi)])
                        prk.append(("PR", ob, gi))
                        continue
                    for sbk in range(2):
                        part = (c0 + sbk * 256) // 256 if gi < 3 else None
                        roped = (gi >= 3) or (part in (0, 2, 4))
                        src = ps[:, sbk * 256:(sbk + 1) * 256]
                        dst = pr[:, oc + sbk * 256: oc + (sbk + 1) * 256]
                        kk = ("PR", ob, gi, sbk)
                        if not roped:
                            P.act(lambda e, src=src, dst=dst: e.copy(dst, src), r=[("ps", pb)], w=[kk])
                            prk.append(kk)
                            continue
                        s4 = src.rearrange("p (h two j) -> p h two j", h=4, two=2)
                        d4 = dst.rearrange("p (h two j) -> p h two j", h=4, two=2)
                        x1 = s4[:, :, 0, :]; x2 = s4[:, :, 1, :]
                        cosb = cs[:, 0:32].unsqueeze(1).broadcast_to([128, 4, 32])
                        sinb = cs[:, 32:64].unsqueeze(1).broadcast_to([128, 4, 32])
                        t1 = T1[:, 0:128].rearrange("p (h j) -> p h j", h=4); t2 = T1[:, 128:256].rearrange("p (h j) -> p h j", h=4)
                        t3 = T2[:, 0:128].rearrange("p (h j) -> p h j", h=4); t4 = T2[:, 128:256].rearrange("p (h j) -> p h j", h=4)
                        P.dve(lambda e, t1=t1, x1=x1, cosb=cosb: e.tensor_tensor(t1, x1, cosb, ALU.mult), r=[("ps", pb), ("CS", ob)], w=["T1a"])
                        P.dve(lambda e, t2=t2, x2=x2, sinb=sinb: e.tensor_tensor(t2, x2, sinb, ALU.mult), r=[("ps", pb), ("CS", ob)], w=["T1b"])
                        P.dve(lambda e, t3=t3, x2=x2, cosb=cosb: e.tensor_tensor(t3, x2, cosb, ALU.mult), r=[("ps", pb), ("CS", ob)], w=["T2a"])
                        P.dve(lambda e, t4=t4, x1=x1, sinb=sinb: e.tensor_tensor(t4, x1, sinb, ALU.mult), r=[("ps", pb), ("CS", ob)], w=["T2b"])
                        P.dve(lambda e, d4=d4, t1=t1, t2=t2: e.tensor_tensor(d4[:, :, 0, :], t1, t2, ALU.subtract), r=["T1a", "T1b"], w=[kk + (0,)])
                        P.dve(lambda e, d4=d4, t3=t3, t4=t4: e.tensor_tensor(d4[:, :, 1, :], t3, t4, ALU.add), r=["T2a", "T2b"], w=[kk + (1,)])
                        prk += [kk + (0,), kk + (1,)]
                PRK[tt] = prk

            def s3_x(tt):
                ob = tt % 2
                pr = PR[ob]; prk = PRK[tt]
                xt = XTt[ob]; vb = VB[0]; gt = GTt[0]
                th = tt // 8; tl = tt % 8
                for idx in range(16):
                    ty, gp = idx // 2, idx % 2
                    b_, s_ = idx // 4, idx % 4
                    c0 = TYCOL[ty] + gp * 128
                    P.pe(lambda e, b_=b_, s_=s_, c0=c0, pr=pr: e.transpose(PS[b_][:, s_ * 128:(s_ + 1) * 128], pr[:, c0:c0 + 128], IDN[:]),
                         r=prk + ["IDN"], w=[("ps", b_)])
                    if s_ == 3:
                        if b_ % 2 == 0:
                            P.act(lambda e, b_=b_, xt=xt: e.copy(xt[:, 4 * b_:4 * b_ + 4, :], PS[b_][:].rearrange("p (s t) -> p s t", s=4)),
                                  r=[("ps", b_)], w=[("XTt", ob, b_)])
                        else:
                            P.dve(lambda e, b_=b_, xt=xt: e.tensor_copy(xt[:, 4 * b_:4 * b_ + 4, :], PS[b_][:].rearrange("p (s t) -> p s t", s=4)),
                                  r=[("ps", b_)], w=[("XTt", ob, b_)])
                for ty in range(8):
                    P.dma("sp", e2x_in[ty][th].ap().rearrange("(gp p) t -> p gp t", p=128)[:, :, tl * 128:(tl + 1) * 128],
                          xt[:, 2 * ty:2 * ty + 2, :], r=[("XTt", ob, ty // 2)], w=[("e2xi", ty, tt)], semkey=("XTst", ob, ty // 2))
                P.pe(lambda e, pr=pr: e.transpose(PS[4][0:48, 0:128], pr[:, 2560:2608], IDN[:]), r=prk + ["IDN"], w=[("ps", 4)])
                P.act(lambda e, gt=gt: e.copy(gt[:], PS[4][0:48, 0:128]), r=[("ps", 4)], w=["GTt"])
                P.dma("sp", e2g_in.ap()[:, tt * 128:(tt + 1) * 128], gt[:], r=["GTt"], w=[("e2gi", tt)], semkey="GTst")
                P.act(lambda e, vb=vb, pr=pr: e.copy(vb[:, 0:256], pr[:, 768:1024]), r=prk, w=[("VB", 0)])
                P.act(lambda e, vb=vb, pr=pr: e.copy(vb[:, 256:512], pr[:, 1280:1536]), r=prk, w=[("VB", 1)])
                P.dma("sp", e2v_in[tt // 4].ap()[(tt % 4) * 128:(tt % 4 + 1) * 128, :], vb[:], r=[("VB", 0), ("VB", 1)],
                      w=[("e2vi", tt)], semkey="VBst")
                if tt in (7, 15):
                    th_ = tt // 8
                    for ty in range(8):
                        P.coll(e2x_in[ty][th_].ap().opt(), e2x_out[ty][th_].ap().opt(), r=[("e2xi", ty, th_ * 8 + i) for i in range(8)],
                               w=[("e2xo", ty, th_)], semkey="cc")
                    for v in (2 * th_, 2 * th_ + 1):
                        P.coll(e2v_in[v].ap().opt(), e2v_out[v].ap().opt(), r=[("e2vi", v * 4 + i) for i in range(4)], w=[("e2vo", v)], semkey="cc")

            if not with_proj:
                for tt in range(NTT):
                    s3_ln(tt)
            else:
                s3_ln(0)
                s3_tr(0)
                for tt in range(NTT):
                    if tt + 1 < NTT:
                        s3_ln(tt + 1)
                    s3_grp(tt)
                    if tt + 1 < NTT:
                        s3_tr(tt + 1)
                    s3_x(tt)
            if with_proj:
                P.coll(e2g_in.ap().opt(), e2g_out.ap().opt(), r=[("e2gi", i) for i in range(16)], w=["e2go"], semkey="cc")

        def phase_attn():
            pfx[0] = "C_"; cur[0] = 0
            KA = sb("KA", [128, 64 * 128], BF16)
            KW = sb("KW", [64, S], BF16)
            VS = sb("VS", [128, 64, 65], BF16); VW = sb("VW", [128, 64, 65], BF16)
            KCM = sb("KCM", [64, 512], BF16); VCM = sb("VCM", [128, 4, 65], BF16)
            OVb = sb("OVb", [128, 4, 128], BF16); CMK = sb("CMK", [128, 16, 2, 128], BF16)
            CAUS = sb("CAUS", [128, 512], BF16); ANTI = sb("ANTI", [128, 512], BF16)
            IDb = sb("IDb", [128, 128], BF16); ID32 = sb("ID32", [128, 128], F32)
            BT = sb("BT", [128, 256], F32)
            ONE32 = sb("ONE32", [128, 64], F32); ONEb = sb("ONEb", [128, 64], BF16)
            RA = [[sb("RA%d%d" % (p, h), [128, 512], BF16) for h in range(2)] for p in range(2)]
            GT = [sb("GT%d" % p, [65, 3, 512], F32) for p in range(2)]
            PC = [sb("PC%d" % i, [128, 512], BF16) for i in range(4)]
            PT = [sb("PT%d" % i, [128, 512], BF16) for i in range(4)]
            DENp = [sb("DEN%d" % p_, [65, 3, 512], F32) for p_ in range(2)]; FACb = sb("FACb", [65, 3, 512], BF16)
            OC = [sb("OC%d" % p_, [64, 512], F32) for p_ in range(3)]
            O3 = [sb("O3_%d" % p_, [65, 512], F32) for p_ in range(2)]; O4 = [sb("O4_%d" % p_, [65, 512], F32) for p_ in range(2)]
            RS = sb("RS", [128, 4], F32); IMPs = sb("IMPs", [128, 128], F32); SC = sb("SC", [128, 128], F32)
            M8 = sb("M8", [128, 16], F32); TMP = sb("TMP", [128, 128], F32)
            SELB = sb("SELB", [128, 128], F32); SB2 = sb("SB2", [128, 128], F32)
            BC = [sb("BC%d" % j, [64, 512], F32) for j in range(3)]
            OACC = [sb("OACC%d" % p, [64, 512], F32) for p in range(2)]
            TMPo = sb("TMPo", [64, 512], F32)
            KC2 = sb("KC2", [128, S + 32], BF16)
            W1 = sb("W1", [128, 16, 256], BF16); W2 = sb("W2", [128, 2, 64], BF16); PE = sb("PE", [128, 16], BF16)
            BIAS = sb("BIAS", [128, 2], F32); HID = sb("HID", [128, 2, 512], BF16)

            ch = lambda ap, b: ap.rearrange("p (a b) -> p a b", b=b)

            def xsrc(ty, th, g):
                return e2x_out[ty][th].ap().rearrange("(k f) t -> f k t", k=4)[ds(g * 64, 64), :, :]

            def kdst(T_, p0, th):
                return T_[p0:p0 + 64, 0:S].rearrange("p (k h t) -> p k h t", k=4, h=2)[:, :, th, :]

            n_ = 0
            for ty in range(8):
                for th in range(2):
                    P.dma_dyn("pool" if n_ % 2 == 0 else "sp",
                              lambda g, ty=ty, th=th: (locx[ty][th].ap().rearrange("d (k t) -> d k t", k=4), xsrc(ty, th, g)),
                              w=[("lx", ty, th)], semkey=("lx", n_ % 4))
                    n_ += 1
            P.dma_dyn("sp", lambda g: (locg.ap().rearrange("(o k rj) t -> o k (rj t)", o=1, k=4),
                                       e2g_out.ap().rearrange("(k g rj) t -> g k (rj t)", k=4, g=4)[ds(g, 1), :, :]),
                      w=["lg"], semkey="lg")

            def lsrc(ty, th):
                return locx[ty][th].ap().rearrange("d (k t) -> d k t", k=4)
            for th in range(2):
                P.dma("pool", kdst(KA, 0, th), lsrc(2, th), r=[("lx", 2, th)], w=[("KAk", th)], semkey="KAk")
                P.dma("sp", kdst(KW, 0, th), lsrc(3, th), r=[("lx", 3, th)], w=[("KW", th)], semkey="KW")
            KAk = [("KAk", 0), ("KAk", 1)]; KWk = [("KW", 0), ("KW", 1)]
            for hh in range(2):
                P.dma("pool", ch(KA[64:128, hh * 4096:(hh + 1) * 4096], 2048), ch(ind_d, 2048), w=[("KAi", hh)], semkey=("KAi", hh))
            P.dve(lambda e: e.memset(VS[:], 1.0), w=["VS"])
            P.dve(lambda e: e.memset(VW[:], 1.0), w=["VW"])
            P.dve(lambda e: e.memset(VCM[:], 1.0), w=["VCM"])
            P.dve(lambda e: e.memset(ONE32[:], 1.0), w=["ONE32"])
            P.dve(lambda e: e.memset(ONEb[:], 1.0), w=["ONEb"])
            P.dve(lambda e: e.memset(SB2[:], 0.0), w=["SB2"])
            P.dve(lambda e: e.memset(KC2[0:64, S:S + 32], 0.0), w=["KC2z0"])
            P.dve(lambda e: e.memset(KC2[64:128, S - 1:S + 32], 0.0), w=["KC2z1"])
            for v in range(4):
                for k in range(4):
                    for (T_, name, c0) in ((VS, "VS", 0), (VW, "VW", 256)):
                        def f(g, T_=T_, v=v, c0=c0, k=k):
                            src = e2v_out[v].ap().rearrange("(k kk p) c -> p k kk c", k=4, kk=4)[:, k, :, ds(c0 + g * 64, 64)]
                            kt0 = k * 16 + v * 4
                            return T_[:, kt0:kt0 + 4, 0:64], src
                        P.dma_dyn("sp" if k % 2 == 0 else "pool", f, r=[name], w=[name], semkey=(name, k % 2))
            P.dma("pool", OVb[:], ov_d.rearrange("(k p) n -> p k n", p=128), w=["OVb"], semkey="OVb")
            P.dma("pool", ch(CMK[:].rearrange("p a b c -> p (a b c)"), 2048), ch(cmk_d, 2048), w=["CMK"], semkey="CMK")
            P.dma("pool", CAUS[:], caus_d, w=["CAUS"], semkey="CAUS")
            P.dma("pool", ANTI[:], anti_d, w=["ANTI"], semkey="ANTI")
            P.dma("pool", IDb[:], ident_d, w=["IDb"], semkey="IDb")
            P.dma("sp", ID32[:], ident_d, w=["ID32"], semkey="ID32")
            P.dma("sp", BT[:], bt_d, w=["BT"], semkey="BT")

            for which in range(2):
                w1 = w1k if which == 0 else w1v
                w2 = w2k if which == 0 else w2v
                pe = pek if which == 0 else pev
                kc2keys = []
                for th in range(2):
                    P.dma("sp", kdst(KC2, 0, th), lsrc(which, th), r=["KC2z0", "KC2z1", ("lx", which, th)],
                          w=[("KC2", 0, th)], semkey=("KC2", 0))
                    kc2keys.append(("KC2", 0, th))
                for k in range(4):
                    for th in range(2):
                        c0 = k * 2048 + th * 1024 - 1
                        t0 = 0
                        if c0 < 0:
                            c0 = 0; t0 = 1
                        P.dma("pool", KC2[64:128, c0:c0 + 1024 - t0], locx[which][th].ap()[:, k * 1024 + t0:(k + 1) * 1024],
                              r=["KC2z0", "KC2z1", ("lx", which, th)], w=[("KC2", 1, k, th)], semkey=("KC2", 1))
                        kc2keys.append(("KC2", 1, k, th))
                P.dma("pool", W1[:], w1.rearrange("(j p) n -> p j n", p=128), w=["W1"], semkey="W1")
                P.dma("pool", W2[:], w2.rearrange("(k p) n -> p k n", p=128), w=["W2"], semkey="W2")
                P.dma("pool", PE[:], pe, w=["PE"], semkey="PE")
                for hcn in range(2):
                    for j in range(16):
                        P.pe(lambda e, hcn=hcn, j=j: e.matmul(PS[7][:, hcn:hcn + 1], W1[:, j, hcn * 128:(hcn + 1) * 128], PE[:, j:j + 1],
                                                              start=(j == 0), stop=(j == 15)), r=["W1", "PE"], w=[("ps", 7)])
                P.dve(lambda e: e.tensor_copy(BIAS[:], PS[7][:, 0:2]), r=[("ps", 7)], w=["BIAS"])
                for hcn in range(2):
                    for j in range(16):
                        rhs = KC2[:, 2 * j: 2 * j + 16 * 512].rearrange("p (n s) -> p n s", s=16)[:, :, 0]
                        P.pe(lambda e, hcn=hcn, j=j, rhs=rhs: e.matmul(PS[hcn][:], W1[:, j, hcn * 128:(hcn + 1) * 128], rhs,
                                                                        start=(j == 0), stop=(j == 15)),
                             r=["W1", "KC2z0", "KC2z1"] + kc2keys, w=[("ps", hcn)])
                    P.act(lambda e, hcn=hcn: e.activation(out=HID[:, hcn, :], in_=PS[hcn][:], func=AF.Gelu_apprx_tanh, bias=BIAS[:, hcn:hcn + 1]),
                          r=[("ps", hcn), "BIAS"], w=[("HID", hcn)])
                if which == 0:
                    for hcn in range(2):
                        P.pe(lambda e, hcn=hcn: e.matmul(PS[2][0:64, :], W2[:, hcn, :], HID[:, hcn, :], start=(hcn == 0), stop=(hcn == 1)),
                             r=["W2", ("HID", hcn)], w=[("ps", 2)])
                    P.act(lambda e: e.copy(KCM[:], PS[2][0:64, :]), r=[("ps", 2)], w=["KCM"])
                else:
                    for nt in range(4):
                        for hcn in range(2):
                            P.pe(lambda e, hcn=hcn, nt=nt: e.matmul(PS[3][:, nt * 64:(nt + 1) * 64], HID[:, hcn, nt * 128:(nt + 1) * 128], W2[:, hcn, :],
                                                                     start=(hcn == 0), stop=(hcn == 1)), r=["W2", ("HID", hcn)], w=[("ps", 3)])
                    P.act(lambda e: e.copy(VCM[:, :, 0:64], PS[3][:, 0:256].rearrange("p (a b) -> p a b", a=4)), r=[("ps", 3), "VCM"], w=["VCM"])
            P.barrier()

            sctr = [0]

            def score_tile(mm_list, dst, dst_key, extra_r):
                b = (0, 1, 6)[sctr[0] % 3]
                sctr[0] += 1
                n = len(mm_list)
                for ii, (l, r_, ks) in enumerate(mm_list):
                    P.pe(lambda e, l=l, r_=r_, ii=ii, b=b: e.matmul(PS[b][:], l, r_, start=(ii == 0), stop=(ii == n - 1)),
                         r=ks, w=[("ps", b)])
                P.act(lambda e, b=b, dst=dst: e.activation(out=dst[:], in_=PS[b][:], func=AF.Exp, scale=0.125),
                      r=[("ps", b)] + list(extra_r), w=[dst_key])

            ptc = [0]

            def fa(i, par, oc):
                nh = 2 if i >= 32 else 1
                kr = i // 16; th = (i % 16) // 8; t0 = ((i % 16) % 8) * 128
                DENi = DENp[par]
                for h in range(nh):
                    for r in range(4):
                        P.dma("pool" if r % 2 == 0 else "sp", RA[par][h][0:64, r * 128:(r + 1) * 128],
                              locx[4 + r][th].ap()[:, kr * 1024 + t0:kr * 1024 + t0 + 128],
                              r=[("lx", 4 + r, th)], w=[("RAq", par, h, r)], semkey=("RAq", par, h, r % 2))
                P.dma("sp", GT[par][64:65, :, :].rearrange("p j (r q) -> p j r q", r=4),
                      locg.ap().rearrange("(k r j) t -> k j r t", k=4, r=4)[kr:kr + 1, :, :, (i % 16) * 128:(i % 16 + 1) * 128],
                      r=["lg"], w=[("GT", par)], semkey=("GT", par))
                QA = RA[par][0][0:64, :]
                qk = [("RAq", par, 0, r) for r in range(4)]
                ctl = (8 * i + 6) // 128
                nct = ctl + 1
                pat = i % 16
                for ct in range(nct):
                    score_tile([(KCM[:, ct * 128:(ct + 1) * 128], QA, ["KCM"] + qk)], PC[ct], ("PC", ct), [])
                    if ct == ctl or (ct == ctl - 1 and pat == 0):
                        wh = 0 if ct == ctl else 1
                        P.dve(lambda e, ct=ct, wh=wh: e.tensor_tensor(PC[ct][:].rearrange("p (r q) -> p r q", r=4),
                                                                       PC[ct][:].rearrange("p (r q) -> p r q", r=4),
                                                                       CMK[:, pat, wh, :].unsqueeze(1).broadcast_to([128, 4, 128]), ALU.mult),
                              r=[("PC", ct), "CMK"], w=[("PC", ct)])
                for ct in range(nct):
                    P.pe(lambda e, ct=ct: e.matmul(PS[2][0:65, :], VCM[:, ct, :], PC[ct][:], start=(ct == 0), stop=(ct == nct - 1)),
                         r=["VCM", ("PC", ct)], w=[("ps", 2)])
                for r in range(4):
                    for ct in range(nct):
                        P.pe(lambda e, ct=ct, r=r: e.matmul(PS[5][:, r * 128:(r + 1) * 128], PC[ct][:, r * 128:(r + 1) * 128], OVb[:, ct, :],
                                                            start=(ct == 0), stop=(ct == nct - 1)), r=[("PC", ct), "OVb"], w=[("ps", 5)])
                P.dve(lambda e: e.tensor_scalar(DENi[64:65, 0, :], PS[2][64:65, :], 1e-30, None, ALU.max), r=[("ps", 2)], w=[("DEN", par, 0)])
                P.act(lambda e: e.copy(OC[oc][:], PS[2][0:64, :]), r=[("ps", 2)], w=[("OC", oc)])

            def fb(i, par):
                DENi = DENp[par]
                for r in range(4):
                    P.pe(lambda e, r=r: e.matmul(PS[7][:, r:r + 1], DENi[64:65, 0, r * 128:(r + 1) * 128], ONE32[64:65, 0:1], start=True, stop=True),
                         r=[("DEN", par, 0), "ONE32"], w=[("ps", 7)])
                P.dve(lambda e: e.reciprocal(RS[:], PS[7][:, 0:4]), r=[("ps", 7)], w=["RS"])
                P.dve(lambda e: e.tensor_scalar(IMPs[:], PS[5][:, 0:128], RS[:, 0:1], None, ALU.mult), r=[("ps", 5), "RS"], w=["IMPs"])
                for r in range(1, 4):
                    P.dve(lambda e, r=r: e.scalar_tensor_tensor(IMPs[:], PS[5][:, r * 128:(r + 1) * 128], RS[:, r:r + 1], IMPs[:], ALU.mult, ALU.add),
                          r=[("ps", 5), "RS", "IMPs"], w=["IMPs"])
                P.dve(lambda e: e.tensor_tensor(SC[:], IMPs[:], BT[:, 128 - 2 * i:256 - 2 * i], ALU.add), r=["IMPs", "BT"], w=["SC"])
                if i >= 1:
                    P.dve(lambda e: e.tensor_scalar(SC[:, 0:1], SC[:, 0:1], 1000.0, None, ALU.add), r=["SC"], w=["SC"])
                P.dve(lambda e: e.max(M8[:, 0:8], SC[:]), r=["SC"], w=["M8a"])
                P.dve(lambda e: e.match_replace(TMP[:], M8[:, 0:8], SC[:], -3.0e38), r=["SC", "M8a"], w=["TMP"])
                P.dve(lambda e: e.max(M8[:, 8:16], TMP[:]), r=["TMP"], w=["M8b"])
                P.dve(lambda e: e.tensor_scalar(SELB[:], SC[:], M8[:, 15:16], NEGB, ALU.is_lt, ALU.mult), r=["SC", "M8b"], w=["SELB"])
                P.dve(lambda e: e.tensor_copy(SB2[:, 64:128], SELB[:, 0:64]), r=["SELB"], w=["SB2"])

            def fc(i, par):
                nh = 2 if i >= 32 else 1
                P.pe(lambda e: e.transpose(PS[7][:, 128:256], SB2[:], ID32[:]), r=["SB2", "ID32"], w=[("ps", 7)])
                P.act(lambda e: e.copy(RA[par][0][64:128, :].rearrange("p (r q) -> p r q", r=4),
                                       PS[7][64:128, 128:256].unsqueeze(1).broadcast_to([64, 4, 128])), r=[("ps", 7)], w=[("RAs", par, 0)])
                if nh == 2:
                    P.pe(lambda e: e.transpose(PS[7][:, 256:384], SELB[:], ID32[:]), r=["SELB", "ID32"], w=[("ps", 7)])
                    P.act(lambda e: e.copy(RA[par][1][64:128, :].rearrange("p (r q) -> p r q", r=4),
                                           PS[7][64:128, 256:384].unsqueeze(1).broadcast_to([64, 4, 128])), r=[("ps", 7)], w=[("RAs", par, 1)])

            def comb_a(i, par):
                DENi = DENp[par]
                P.dve(lambda e: e.tensor_scalar(DENi[64:65, 1, :], O3[par][64:65, :], 1e-30, None, ALU.max), r=[("O3", par)], w=[("DEN", par, 1)])
                P.dve(lambda e: e.tensor_scalar(DENi[64:65, 2, :], O4[par][64:65, :], 1e-30, None, ALU.max), r=[("O4", par)], w=[("DEN", par, 2)])
                dk = [("DEN", par, 0), ("DEN", par, 1), ("DEN", par, 2)]
                P.dve(lambda e: e.reciprocal(DENi[64:65, :, :], DENi[64:65, :, :]), r=dk, w=dk)
                P.dve(lambda e: e.tensor_tensor(FACb[64:65, :, :], DENi[64:65, :, :], GT[par][64:65, :, :], ALU.mult),
                      r=dk + [("GT", par)], w=["FACb"])

            def comb_b(i, par, oc):
                oa = OACC[par]
                for j in range(3):
                    P.pe(lambda e, j=j: e.matmul(PS[7][0:64, :], ONEb[64:65, 0:64], FACb[64:65, j, :], start=True, stop=True),
                         r=["ONEb", "FACb"], w=[("ps", 7)])
                    if j == 0:
                        P.dve(lambda e, oa=oa: e.tensor_tensor(oa[:], PS[7][0:64, :], OC[oc][:], ALU.mult), r=[("OC", oc), ("ps", 7)], w=[("OACC", par)])
                    else:
                        src = (O3 if j == 1 else O4)[par][0:64, :]
                        skey = ("O3" if j == 1 else "O4", par)
                        P.dve(lambda e, src=src, j=j: e.tensor_tensor(TMPo[:], PS[7][0:64, :], src, ALU.mult), r=[skey, ("ps", 7)], w=["TMPo"])
                        P.dve(lambda e, oa=oa: e.tensor_tensor(oa[:], oa[:], TMPo[:], ALU.add), r=[("OACC", par), "TMPo"], w=[("OACC", par)])
                c = i % 16; qq = i // 16
                P.dma("pool", e3_in[c].ap().rearrange("(r d) t -> d r t", d=64)[:, :, qq * 128:(qq + 1) * 128],
                      oa[:].rearrange("p (r q) -> p r q", r=4), r=[("OACC", par)], w=[("e3i", i)], semkey=("OACC", par))

            def make_tiles(i, par):
                QA = RA[par][0][0:64, :]
                qk = [("RAq", par, 0, r) for r in range(4)]
                tiles = []
                k0 = max(0, i - 4)
                for kt in range(k0, i + 1):
                    mm = [(KW[:, kt * 128:(kt + 1) * 128], QA, KWk + qk)]
                    if kt == i:
                        mm.append((IDb[:], CAUS[:], ["IDb", "CAUS"]))
                    elif kt == i - 4:
                        mm.append((IDb[:], ANTI[:], ["IDb", "ANTI"]))
                    tiles.append((mm, 4, VW, "VW", kt, kt == k0, kt == i))
                for kt in range(i + 1):
                    h = kt // 32
                    mm = [(KA[:, kt * 128:(kt + 1) * 128], RA[par][h][:],
                           KAk + [("KAi", h), ("RAs", par, h)] + [("RAq", par, h, r) for r in range(4)])]
                    if kt == i:
                        mm.append((IDb[:], CAUS[:], ["IDb", "CAUS"]))
                    tiles.append((mm, 3, VS, "VS", kt, kt == 0, kt == i))
                return tiles

            order = [qq_ * 16 + c_ for c_ in range(16) for qq_ in range(4)]
            fa(order[0], 0, 0); fb(order[0], 0); fc(order[0], 0)
            for n_ in range(NQB):
                i = order[n_]; par = n_ % 2
                if n_ >= 1:
                    comb_a(order[n_ - 1], (n_ - 1) % 2)
                if n_ + 1 < NQB:
                    fa(order[n_ + 1], (n_ + 1) % 2, (n_ + 1) % 3)
                tiles = make_tiles(i, par)
                T_ = len(tiles)
                pbs = {}

                def emit_score(j):
                    pb = ptc[0] % 4
                    ptc[0] += 1
                    pbs[j] = pb
                    score_tile(tiles[j][0], PT[pb], ("PT", pb), [])

                def emit_pv(j):
                    mm, bank, Vt, vkey, kt, st, sp_ = tiles[j]
                    pb = pbs[j]
                    P.pe(lambda e, kt=kt, pb=pb, bank=bank, Vt=Vt, st=st, sp_=sp_: e.matmul(PS[bank][0:65, :], Vt[:, kt, :], PT[pb][:], start=st, stop=sp_),
                         r=[vkey, ("PT", pb)], w=[("ps", bank)])
                emit_score(0)
                emit_score(1)
                for j in range(T_):
                    if j + 2 < T_:
                        emit_score(j + 2)
                    emit_pv(j)
                    if j == min(1, T_ - 1) and n_ >= 1:
                        comb_b(order[n_ - 1], (n_ - 1) % 2, (n_ - 1) % 3)
                        if (n_ - 1) % 4 == 3:
                            c = (n_ - 1) // 4
                            P.coll(e3_in[c].ap().opt(), e3_out[c].ap().opt(), r=[("e3i", c + 16 * q_) for q_ in range(4)], w=[("e3o", c)], semkey="cc")
                    if n_ + 1 < NQB:
                        if j == T_ // 3:
                            fb(order[n_ + 1], (n_ + 1) % 2)
                        if j == (2 * T_) // 3:
                            fc(order[n_ + 1], (n_ + 1) % 2)
                P.act(lambda e, par=par: e.copy(O3[par][:], PS[3][0:65, :]), r=[("ps", 3)], w=[("O3", par)])
                P.dve(lambda e, par=par: e.tensor_copy(O4[par][:], PS[4][0:65, :]), r=[("ps", 4)], w=[("O4", par)])
            comb_a(order[NQB - 1], (NQB - 1) % 2)
            comb_b(order[NQB - 1], (NQB - 1) % 2, (NQB - 1) % 3)
            P.coll(e3_in[15].ap().opt(), e3_out[15].ap().opt(), r=[("e3i", 15 + 16 * q_) for q_ in range(4)], w=[("e3o", 15)], semkey="cc")

        phase_ret()
        if STOP != "A":
            P.new_phase()
            phase_post(0, 16, True,
                   lambda tt, g: e1_out[tt // 2].ap().rearrange("(k p) t -> p k t", p=128)[:, :, ds(g * 256 + (tt % 2) * 128, 128)],
                   "e1o", xres, h1d.ap(), wout0)
        if STOP not in ("A", "B"):
            P.new_phase()
            phase_attn()
        if STOP not in ("A", "B", "C"):
            P.new_phase()
            phase_post(1, 8, False,
                   lambda tt, g: e3_out[tt].ap().rearrange("(k p) t -> p k t", p=128)[:, :, ds(g * 128, 128)],
                   "e3o", h1d.ap(), hout, wout1)
        P.emit()
    return nc


def ret_consts(hd):
    dk, C = 256, 128
    lg = np.float32(np.log1p(-(2.0 ** (-5.0 - hd))))
    idx = np.arange(C, dtype=np.float32)
    diff = idx[:, None] - idx[None, :]
    dmask = np.where(diff >= 0, np.exp(lg * np.maximum(diff, 0.0)), 0.0).astype(np.float32)
    dmT = np.ascontiguousarray(dmask.T) * np.float32(dk ** -0.5)
    qdec = np.exp(lg * (idx + 1.0)).astype(np.float32)
    kdec = (np.exp(lg * (C - 1.0 - idx)) * np.float32(dk ** -0.5)).astype(np.float32)
    cdec = np.float32(np.exp(lg * C))
    return dict(dm=dmT.astype(np.float32), qd=np.tile(qdec[None, :], (128, 1)).astype(np.float32),
                kd=kdec[:, None].astype(np.float32), cd=np.full((128, 1), cdec, np.float32))


def ret_tables():
    inv = (10000.0 ** (-np.arange(0, 256, 2, dtype=np.float32) / 256)).astype(np.float32)
    ang = np.arange(S, dtype=np.float32)[None, :] * inv[:, None]
    return np.cos(ang).astype(np.float32), np.sin(ang).astype(np.float32)


def attn_consts():
    ind = np.zeros((64, 32 * 128), np.float32)
    for kt in range(32):
        for key in range(128):
            ind[2 * kt + key // 64, kt * 128 + key] = 1.0
    kk = np.arange(128)[:, None]; qq = np.arange(128)[None, :]
    caus = np.where(kk > qq, NEGB, 0.0).astype(np.float32)
    anti = np.where(kk <= qq, NEGB, 0.0).astype(np.float32)
    caus4 = np.tile(caus, (1, 4)); anti4 = np.tile(anti, (1, 4))
    cmk = np.zeros((128, 16, 2, 128), np.float32)
    m = np.arange(128)[:, None]; tp = np.arange(128)[None, :]
    for p in range(16):
        cmk[:, p, 0, :] = (16 * (m - 8 * p) + 31 <= tp)
        cmk[:, p, 1, :] = (16 * (m - 128 - 8 * p) + 31 <= tp)
    ci = np.arange(512)[:, None]; sj = np.arange(128)[None, :]
    ov = ((ci * 16 < (sj + 1) * 64) & (ci * 16 + 32 > sj * 64) & (ci < 511)).astype(np.float32)
    bt = np.zeros((128, 256), np.float32)
    for t in range(128):
        cr = 1 if t >= 64 else 0
        for c in range(256):
            d = c - 128
            if d == cr or d == cr - 1:
                bt[t, c] = 1000.0
            elif d > cr:
                bt[t, c] = -1e30
    return dict(ind=ind, caus4=caus4, anti4=anti4, cmk=cmk.reshape(128, -1), ov=ov, bt=bt)


_CACHE = {}


def kernel(**inputs):
    inp = {k: np.ascontiguousarray(np.asarray(v, dtype=np.float32)) for k, v in inputs.items()}
    x = inp["x"]
    B, S_, D = x.shape
    T = B * S_
    cores = list(range(8))
    if "nc" not in _CACHE:
        _CACHE["nc"] = build_fused()
    nc = _CACHE["nc"]
    w_in = inp["ret_w_in"][0]
    cosT, sinT = ret_tables()
    sel = np.zeros((16, 16 * 128), np.float32)
    for e in range(16):
        sel[e, e * 128:(e + 1) * 128] = 1.0
    pos = (np.arange(T) % S_).astype(np.float32)
    inv = (10000.0 ** (-np.arange(0, 64, 2, dtype=np.float32) / 64)).astype(np.float32)
    ang = pos[:, None] * inv[None, :]
    cs_full = np.concatenate([np.cos(ang), np.sin(ang)], 1).astype(np.float32)
    win = inp["nsa_w_in"][0]
    perm = np.array([(g * 4 + r) * 64 + d for r in range(4) for g in range(4) for d in range(64)])
    wp = np.ascontiguousarray(np.concatenate([inp["nsa_w_kv"], win[:, perm], win[:, 1024:1072]], 1))
    lnp = np.ascontiguousarray(np.stack([inp["ln_mix_g"][0], inp["ln_mix_b"][0], inp["ln_ffn_g"][0], inp["ln_ffn_b"][0],
                                         inp["ln_mix_g"][1], inp["ln_mix_b"][1], inp["ln_ffn_g"][1], inp["ln_ffn_b"][1]], 0))
    pe2 = lambda pe: np.ascontiguousarray(pe.reshape(16, 2, 64).transpose(1, 2, 0).reshape(128, 16))
    shared = dict(cosT=cosT, sinT=sinT, ident=np.eye(128, dtype=np.float32), sel=sel,
                  wout0=inp["ret_w_out"][0], wout1=inp["nsa_w_out"][0], lnp=lnp,
                  rw=inp["router_w"], rbias=inp["router_b"][None],
                  wg=inp["moe_w_gate"], wu=inp["moe_w_up"], wd=inp["moe_w_down"], wp=wp,
                  w1k=inp["cmp_k_w1"], w2k=inp["cmp_k_w2"], w1v=inp["cmp_v_w1"], w2v=inp["cmp_v_w2"],
                  pek=pe2(inp["cmp_pe_k"]), pev=pe2(inp["cmp_pe_v"]))
    shared.update(attn_consts())
    xf = x.reshape(T, D)
    xTs = [np.ascontiguousarray(x[b].T) for b in range(B)]
    maps = []
    for c in cores:
        b, hd = c // 4, c % 4
        sl = slice(c * NT, (c + 1) * NT)
        wr = np.ascontiguousarray(np.concatenate([
            w_in[:, hd * 256:(hd + 1) * 256], w_in[:, 1024 + hd * 256:1024 + (hd + 1) * 256],
            w_in[:, 2048 + hd * 512:2048 + (hd + 1) * 512], w_in[:, 4096 + hd * 512:4096 + (hd + 1) * 512]], 1))
        m = dict(shared)
        m.update(xT=xTs[b], wr=wr, xres=np.ascontiguousarray(xf[sl]), cs=np.ascontiguousarray(cs_full[sl]))
        m.update(ret_consts(hd))
        maps.append(m)
    res = run_bass_kernel_spmd(nc, maps, core_ids=cores)
    _LAST["res"] = res
    out = np.concatenate([r["hout"] for r in res.results], 0).reshape(B, S_, D)
    return out.astype(np.float32)
```

```python
from contextlib import ExitStack
import numpy as np
import concourse.bass as bass
import concourse.mybir as mybir
from concourse.alu_op_type import AluOpType as ALU
from concourse.bass_utils import run_bass_kernel_spmd

F32 = mybir.dt.float32
BF16 = mybir.dt.bfloat16
AF = mybir.ActivationFunctionType
AX = mybir.AxisListType

SAME_ENG_SYNC = True
STOP = ""
DBGOUT = False
_LAST = {}
GROUPS = [[0, 1, 2, 3], [4, 5, 6, 7]]


class _Op:
    __slots__ = ("eng", "fn", "deps", "dma", "semkey", "needs", "sem", "sigval", "waits", "final", "inc")

    def __init__(self, eng, fn, dma=False, semkey=None, inc=16):
        self.eng = eng
        self.fn = fn
        self.deps = set()
        self.dma = dma
        self.semkey = semkey
        self.needs = False
        self.sem = None
        self.sigval = 0
        self.waits = []
        self.final = False
        self.inc = inc


class Arena:
    def __init__(self, nc, nbytes):
        self.nc = nc
        self.beg, self.end = nc.bump_sbuf(nbytes)
        self.nbytes = nbytes
        self.n = 0

    def at(self, name, shape, dtype, off):
        sz = 1
        for d in shape[1:]:
            sz *= d
        sz *= mybir.dt.size(dtype) if hasattr(mybir.dt, "size") else {F32: 4, BF16: 2}[dtype]
        assert off + sz <= self.nbytes, (name, off, sz, self.nbytes)
        self.n += 1
        return self.nc.alloc_sbuf_tensor_at(name, shape, dtype, offset=self.beg + off)


class Prog:
    ENGS = ("pe", "act", "dve", "pool", "sp")

    def __init__(self, nc):
        self.nc = nc
        self.ops = []
        self.last_w = {}
        self.readers = {}
        self.pend_bar = {}
        self.last_eng = {}
        self.last_dma = {}
        self.slotmap = {}
        self.dynval = {}

    def barrier(self):
        allp = set(self.last_eng.values()) | set(self.last_dma.values())
        for e in self.ENGS:
            self.pend_bar[e] = set(allp) | self.pend_bar.get(e, set())

    def new_phase(self):
        self.barrier()
        self.slotmap = {}
        self.last_w = {}
        self.readers = {}

    def add(self, eng, fn, r=(), w=(), dma=False, semkey=None, inc=16):
        if dma:
            kind = "cc" if inc != 16 else eng
            if (kind, semkey) not in self.slotmap:
                n_kind = sum(1 for k in self.slotmap if k[0] == kind)
                self.slotmap[(kind, semkey)] = (kind, n_kind)
            semkey = self.slotmap[(kind, semkey)]
        op = _Op(eng, fn, dma, semkey, inc)
        idx = len(self.ops)
        deps = set()
        xr = [k for k in r if isinstance(k, tuple) and k[0] == "ps"]
        if xr:
            w = list(w) + [k for k in xr if k not in w]
        for k in r:
            lw = self.last_w.get(k)
            if lw is not None:
                deps.add(lw)
        for k in w:
            lw = self.last_w.get(k)
            if lw is not None:
                deps.add(lw)
            for rd in self.readers.get(k, ()):
                deps.add(rd)
        best = {}
        for d in deps:
            p = self.ops[d]
            if p.dma:
                d = self.last_dma[p.semkey]
                p = self.ops[d]
            if (not p.dma) and p.eng == eng and (eng == "pe" or not SAME_ENG_SYNC):
                continue
            bk = ("d", p.semkey) if p.dma else ("e", p.eng)
            if bk not in best or best[bk] < d:
                best[bk] = d
        for d in best.values():
            op.deps.add(d)
            self.ops[d].needs = True
        for d in self.pend_bar.pop(eng, ()):
            p = self.ops[d]
            if (not p.dma) and p.eng == eng:
                continue
            op.deps.add(d)
            p.needs = True
        self.ops.append(op)
        if dma:
            self.last_dma[semkey] = idx
        else:
            self.last_eng[eng] = idx
        for k in w:
            self.last_w[k] = idx
            self.readers[k] = []
        for k in r:
            if k in w:
                continue
            self.readers.setdefault(k, []).append(idx)
        return idx

    def pe(self, fn, r=(), w=()):
        return self.add("pe", fn, r, w)

    def act(self, fn, r=(), w=()):
        return self.add("act", fn, r, w)

    def dve(self, fn, r=(), w=()):
        return self.add("dve", fn, r, w)

    def pool(self, fn, r=(), w=()):
        return self.add("pool", fn, r, w)

    def dma(self, q, out, in_, r=(), w=(), semkey=None, **kw):
        assert semkey is not None
        return self.add(q, lambda e: e.dma_start(out=out, in_=in_, **kw), r, w, dma=True, semkey=semkey)

    def dma_dyn(self, q, fn, r=(), w=(), semkey=None, **kw):
        assert semkey is not None

        def f(e):
            o, i = fn(self.dynval[q])
            return e.dma_start(out=o, in_=i, **kw)
        return self.add(q, f, r, w, dma=True, semkey=semkey)

    def coll(self, in_ap, out_ap, r=(), w=(), semkey="cc"):
        return self.add("pool", lambda e: e.collective_compute(
            "AllGather", mybir.AluOpType.bypass, replica_groups=GROUPS, ins=[in_ap], outs=[out_ap]),
            r, w, dma=True, semkey=semkey, inc=1)

    def emit(self):
        nc = self.nc
        with ExitStack() as es:
            esem = {e: es.enter_context(nc.semaphore("c_" + e)) for e in ("pe", "act", "dve", "pool")}
            dsem = {}
            ecnt = {e: 0 for e in esem}
            dcnt = {}
            known = {e: {} for e in self.ENGS}
            for op in self.ops:
                if op.dma:
                    if op.semkey not in dsem:
                        dsem[op.semkey] = es.enter_context(nc.semaphore("d%d" % len(dsem)))
                        dcnt[op.semkey] = 0
                    dcnt[op.semkey] += op.inc
                    op.sem = dsem[op.semkey]
                    op.sigval = dcnt[op.semkey]
                elif op.needs:
                    ecnt[op.eng] += 1
                    op.sem = esem[op.eng]
                    op.sigval = ecnt[op.eng]
                need = {}
                for d in op.deps:
                    p = self.ops[d]
                    key = id(p.sem)
                    if key not in need or need[key][1] < p.sigval:
                        need[key] = (p.sem, p.sigval)
                kn = known[op.eng]
                for key, (sem, val) in need.items():
                    if kn.get(key, 0) >= val:
                        continue
                    kn[key] = val
                    op.waits.append((sem, val))
            print("[prog] ops=%d sems=%d eng_counts=%s max_dma_cnt=%d" % (
                len(self.ops), len(dsem) + 4, ecnt, max(dcnt.values()) if dcnt else 0), flush=True)
            finals = [(dsem[k], dcnt[k]) for k in dsem]
            byeng = {e: [op for op in self.ops if op.eng == e] for e in self.ENGS}
            with nc.Block() as block:
                def run(eng, name, is_last):
                    if name == "sp":
                        self.dynval[name] = nc.sync.partition_id() % 4
                    elif name == "pool":
                        self.dynval[name] = nc.gpsimd.partition_id() % 4
                    for op in byeng[name]:
                        for sem, val in op.waits:
                            eng.wait_ge(sem, val)
                        inst = op.fn(eng)
                        if op.dma:
                            inst.then_inc(op.sem, op.inc)
                        elif op.needs:
                            inst.then_inc(op.sem, 1)
                    if is_last:
                        for sem, val in finals:
                            eng.wait_ge(sem, val)

                @block.tensor
                def _(e):
                    run(e, "pe", False)

                @block.scalar
                def _(e):
                    run(e, "act", False)

                @block.vector
                def _(e):
                    run(e, "dve", False)

                @block.gpsimd
                def _(e):
                    run(e, "pool", False)

                @block.sync
                def _(e):
                    run(e, "sp", True)


ALPHA = 4.0 ** 0.25
LN_EPS = 1e-5
NT = 2048
NTT = NT // 128
NE = 16
S = 8192
NCH = 64
NEGB = -30000.0
NQB = 64
TYCOL = [0, 256, 512, 1024, 1536, 1792, 2048, 2304]


def build_fused():
    nc = bass.Bass("TRN2", target_bir_lowering=False)
    din = lambda n, s: nc.dram_tensor(n, s, F32, kind="ExternalInput").ap()
    xT = din("xT", [1024, S]); wr = din("wr", [1024, 1536])
    cosT = din("cosT", [128, S]); sinT = din("sinT", [128, S])
    dm_d = din("dm", [128, 128]); qd_d = din("qd", [128, 128]); kd_d = din("kd", [128, 1]); cd_d = din("cd", [128, 1])
    ident_d = din("ident", [128, 128]); sel_d = din("sel", [16, NE * 128])
    xres = din("xres", [NT, 1024])
    wout0 = din("wout0", [2048, 1024]); wout1 = din("wout1", [1024, 1024])
    lnp = din("lnp", [8, 1024])
    rw = din("rw", [1024, 16]); rbias = din("rbias", [1, 16])
    wg = din("wg", [2, NE, 1024, 512]); wu = din("wu", [2, NE, 1024, 512]); wd = din("wd", [2, NE, 512, 1024])
    wp = din("wp", [1024, 2608]); cs_d = din("cs", [NT, 64])
    w1k = din("w1k", [2048, 256]); w2k = din("w2k", [256, 64]); w1v = din("w1v", [2048, 256]); w2v = din("w2v", [256, 64])
    pek = din("pek", [128, 16]); pev = din("pev", [128, 16])
    ind_d = din("ind", [64, 32 * 128])
    caus_d = din("caus4", [128, 512]); anti_d = din("anti4", [128, 512])
    cmk_d = din("cmk", [128, 16 * 2 * 128]); ov_d = din("ov", [512, 128]); bt_d = din("bt", [128, 256])
    hout = nc.dram_tensor("hout", [NT, 1024], F32, kind="ExternalOutput").ap()
    idram = lambda n, s, d: nc.dram_tensor(n, s, d)
    e1_in = [idram("e1i%d" % c, [512, 1024], BF16) for c in range(8)]
    e1_out = [idram("e1o%d" % c, [2048, 1024], BF16) for c in range(8)]
    e2x_in = [[idram("e2xi%d_%d" % (ty, th), [256, 1024], BF16) for th in range(2)] for ty in range(8)]
    e2x_out = [[idram("e2xo%d_%d" % (ty, th), [1024, 1024], BF16) for th in range(2)] for ty in range(8)]
    e2v_in = [idram("e2vi%d" % v, [512, 512], BF16) for v in range(4)]
    e2v_out = [idram("e2vo%d" % v, [2048, 512], BF16) for v in range(4)]
    e2g_in = idram("e2gi", [48, NT], F32)
    e2g_out = idram("e2go", [192, NT], F32)
    e3_in = [idram("e3i%d" % c, [256, 512], BF16) for c in range(16)]
    e3_out = [idram("e3o%d" % c, [1024, 512], BF16) for c in range(16)]
    h1d = nc.dram_tensor("h1d", [NT, 1024], F32, kind="ExternalOutput") if DBGOUT else idram("h1d", [NT, 1024], F32)
    xkind = dict(kind="ExternalOutput") if DBGOUT else {}
    locx = [[nc.dram_tensor("lx%d_%d" % (ty, th), [64, 4096], BF16, **xkind) for th in range(2)] for ty in range(8)]
    locg = nc.dram_tensor("lgt", [48, NT], F32, **xkind)
    dbg3 = nc.dram_tensor("dbg3", [16, 256, 512], BF16, kind="ExternalOutput") if DBGOUT else None
    dbg4 = nc.dram_tensor("dbg4", [2, 1024, 512], BF16, kind="ExternalOutput") if DBGOUT else None
    ds = bass.ds

    with ExitStack() as es:
        AR = Arena(nc, 200 * 1024)
        PS = [es.enter_context(nc.psum_tensor("ps%d" % i, [128, 512], F32)) for i in range(8)]
        P = Prog(nc)
        cur = [0]
        pfx = [""]

        def sb(n, s, d, off=None):
            sz = 1
            for x in s[1:]:
                sz *= x
            sz *= (4 if d == F32 else 2)
            sz = (sz + 31) // 32 * 32
            if off is None:
                off = cur[0]
                cur[0] += sz
            return AR.at(pfx[0] + n, s, d, off)

        def phase_ret():
            pfx[0] = "A_"; cur[0] = 0
            W = sb("W", [128, 8, 1536], BF16)
            CT = sb("CT", [128, S], F32); ST = sb("ST", [128, S], F32)
            DM = sb("DM", [128, 128], F32); QD = sb("QD", [128, 128], F32); KDv = sb("KDv", [128, 1], F32); CDv = sb("CDv", [128, 1], F32)
            IDB = sb("IDB", [128, 128], BF16)
            S32 = sb("S32", [128, 2, 512], F32); Sb = sb("Sb", [128, 2, 512], BF16)
            XT = [sb("XT%d" % i, [128, 8, 128], BF16) for i in range(2)]
            T = [sb("T%d" % i, [128, 128], F32) for i in range(4)]
            QT = [sb("QT%d" % i, [128, 2, 128], BF16) for i in range(2)]; KT = [sb("KT%d" % i, [128, 2, 128], BF16) for i in range(2)]
            QDT = [sb("QDT%d" % i, [128, 2, 128], BF16) for i in range(2)]
            V = [sb("V%d" % i, [128, 512], BF16) for i in range(2)]; SGt = [sb("SGt%d" % i, [128, 512], F32) for i in range(2)]
            INT = sb("INT", [128, 128], BF16); KD = sb("KD", [128, 2, 128], BF16)
            ON = sb("ON", [128, 512], F32); YB = [sb("YB%d" % i, [128, 512], BF16) for i in range(2)]
            YTT = [sb("YTT%d" % i, [128, 4, 128], BF16) for i in range(2)]
            st6 = sb("st6", [128, 6], F32); mv = sb("mv", [128, 2], F32); rstd = sb("rstd", [128, 1], F32)
            PSt = PS[7][:].bitcast(BF16)

            P.dma("pool", W[:], wr.rearrange("(k p) n -> p k n", p=128), w=["W"], semkey="W")
            for q4 in range(4):
                P.dma("sp", CT[:, q4 * 2048:(q4 + 1) * 2048], cosT[:, q4 * 2048:(q4 + 1) * 2048], w=[("CT", q4)], semkey=("CT", q4))
                P.dma("sp", ST[:, q4 * 2048:(q4 + 1) * 2048], sinT[:, q4 * 2048:(q4 + 1) * 2048], w=[("ST", q4)], semkey=("ST", q4))
            P.dma("sp", DM[:], dm_d, w=["DM"], semkey="DM")
            P.dma("sp", QD[:], qd_d, w=["QD"], semkey="QD")
            P.dma("sp", KDv[:], kd_d, w=["KDv"], semkey="KDv")
            P.dma("sp", CDv[:], cd_d, w=["CDv"], semkey="CDv")
            P.dma("pool", IDB[:], ident_d, w=["IDB"], semkey="IDB")
            P.dve(lambda e: e.memset(S32[:], 0.0), w=["S32"])
            P.dve(lambda e: e.memset(Sb[:], 0.0), w=["Sb"])
            xTv = xT.rearrange("(k p) t -> p k t", p=128)

            def f_load(n):
                xb = n % 2
                P.dma("pool", XT[xb][:], xTv[:, :, n * 128:(n + 1) * 128], w=[("XT", xb)], semkey=("XT", xb))

            def f_mm(n, gi):
                xb = n % 2
                X = XT[xb]
                if gi < 4:
                    reg = gi
                    for kc in range(8):
                        P.pe(lambda e, reg=reg, kc=kc, X=X: e.matmul(PS[0][:, reg * 128:(reg + 1) * 128], W[:, kc, reg * 128:(reg + 1) * 128],
                                                                      X[:, kc, :], start=(kc == 0), stop=(kc == 7)),
                             r=["W", ("XT", xb)], w=[("ps", 0)])
                else:
                    bank = 1 if gi == 4 else 2
                    c0 = 512 if gi == 4 else 1024
                    for kc in range(8):
                        P.pe(lambda e, kc=kc, X=X, bank=bank, c0=c0: e.matmul(PS[bank][:], X[:, kc, :], W[:, kc, c0:c0 + 512], start=(kc == 0), stop=(kc == 7)),
                             r=["W", ("XT", xb)], w=[("ps", bank)])

            def f_post(n):
                b_ = n % 2
                tsl = slice(n * 128, (n + 1) * 128)
                q4 = n // 16
                C = CT[:, tsl]; Sn = ST[:, tsl]
                for which, dst in ((0, QT[b_]), (1, KT[b_])):
                    A = PS[0][:, (2 * which) * 128:(2 * which + 1) * 128]
                    B = PS[0][:, (2 * which + 1) * 128:(2 * which + 2) * 128]
                    dk = ("QT", b_) if which == 0 else ("KT", b_)
                    P.dve(lambda e, A=A, C=C: e.tensor_tensor(T[0][:], A, C, ALU.mult), r=[("ps", 0), ("CT", q4)], w=["T0"])
                    P.dve(lambda e, B=B, Sn=Sn: e.tensor_tensor(T[1][:], B, Sn, ALU.mult), r=[("ps", 0), ("ST", q4)], w=["T1"])
                    P.dve(lambda e, B=B, C=C: e.tensor_tensor(T[2][:], B, C, ALU.mult), r=[("ps", 0), ("CT", q4)], w=["T2"])
                    P.dve(lambda e, A=A, Sn=Sn: e.tensor_tensor(T[3][:], A, Sn, ALU.mult), r=[("ps", 0), ("ST", q4)], w=["T3"])
                    P.dve(lambda e, dst=dst: e.tensor_tensor(dst[:, 0, :], T[0][:], T[1][:], ALU.subtract), r=["T0", "T1"], w=[dk])
                    P.dve(lambda e, dst=dst: e.tensor_tensor(dst[:, 1, :], T[2][:], T[3][:], ALU.add), r=["T2", "T3"], w=[dk])
                P.dve(lambda e: e.tensor_tensor(QDT[b_][:], QT[b_][:], QD[:].unsqueeze(1).broadcast_to([128, 2, 128]), ALU.mult),
                      r=[("QT", b_), "QD"], w=[("QDT", b_)])
                P.act(lambda e: e.copy(V[b_][:], PS[1][:]), r=[("ps", 1)], w=[("V", b_)])
                P.act(lambda e: e.activation(out=SGt[b_][:], in_=PS[2][:], func=AF.Silu), r=[("ps", 2)], w=[("SGt", b_)])

            def b_s0(n):
                b_ = n % 2
                for dc in range(2):
                    P.pe(lambda e, dc=dc: e.matmul(PS[3][:, 0:128], KT[b_][:, dc, :], QT[b_][:, dc, :], start=(dc == 0), stop=(dc == 1)),
                         r=[("KT", b_), ("QT", b_)], w=[("ps", 3)])
                P.dve(lambda e: e.tensor_tensor(INT[:], PS[3][:, 0:128], DM[:], ALU.mult), r=[("ps", 3), "DM"], w=["INT"])
                for dc in range(2):
                    P.pe(lambda e, dc=dc: e.transpose(PSt[:, dc * 128:(dc + 1) * 128], KT[b_][:, dc, :], IDB[:]), r=[("KT", b_), "IDB"], w=[("ps", 7)])
                P.dve(lambda e: e.tensor_scalar(KD[:], PSt[:, 0:256].rearrange("p (a b) -> p a b", a=2), KDv[:, 0:1], None, ALU.mult),
                      r=[("ps", 7), "KDv"], w=["KD"])

            def b_s1(n):
                b_ = n % 2
                P.pe(lambda e: e.matmul(PS[4][:], INT[:], V[b_][:], start=True, stop=False), r=["INT", ("V", b_)], w=[("ps", 4)])
                for dc in range(2):
                    P.pe(lambda e, dc=dc: e.matmul(PS[4][:], QDT[b_][:, dc, :], Sb[:, dc, :], start=False, stop=(dc == 1)),
                         r=[("QDT", b_), "Sb"], w=[("ps", 4)])
                for dc in range(2):
                    P.pe(lambda e, dc=dc: e.matmul(PS[5 + dc][:], KD[:, dc, :], V[b_][:], start=True, stop=True), r=["KD", ("V", b_)], w=[("ps", 5 + dc)])
                    P.dve(lambda e, dc=dc: e.scalar_tensor_tensor(S32[:, dc, :], S32[:, dc, :], CDv[:, 0:1], PS[5 + dc][:], ALU.mult, ALU.add),
                          r=[("ps", 5 + dc), "S32", "CDv"], w=["S32"])
                P.act(lambda e: e.copy(Sb[:], S32[:]), r=["S32"], w=["Sb"])

            def b_s2(n):
                b_ = n % 2
                P.dve(lambda e: e.bn_stats(st6[:], PS[4][:]), r=[("ps", 4)], w=["st6"])
                P.dve(lambda e: e.bn_aggr(mv[:], st6[:]), r=["st6"], w=["mv"])
                P.dve(lambda e: e.tensor_scalar(rstd[:], mv[:, 1:2], 1e-5, None, ALU.add), r=["mv", "rstd"], w=["rstd"])
                P.act(lambda e: e.activation(out=rstd[:], in_=rstd[:], func=AF.Sqrt), r=["rstd"], w=["rstd"])
                P.dve(lambda e: e.reciprocal(rstd[:], rstd[:]), r=["rstd"], w=["rstd"])
                P.dve(lambda e: e.tensor_scalar(ON[:], PS[4][:], mv[:, 0:1], rstd[:, 0:1], ALU.subtract, ALU.mult),
                      r=[("ps", 4), "mv", "rstd"], w=["ON"])
                yb = YB[n % 2]
                P.dve(lambda e, yb=yb: e.tensor_tensor(yb[:], ON[:], SGt[b_][:], ALU.mult), r=["ON", ("SGt", b_)], w=[("YB", n % 2)])

            def b_s3(n):
                yb = YB[n % 2]; ytt = YTT[n % 2]
                for j in range(4):
                    P.pe(lambda e, j=j, yb=yb: e.transpose(PSt[:, 512 + j * 128:512 + (j + 1) * 128], yb[:, j * 128:(j + 1) * 128], IDB[:]),
                         r=[("YB", n % 2), "IDB"], w=[("ps", 7)])
                P.act(lambda e, ytt=ytt: e.copy(ytt[:], PSt[:, 512:1024].rearrange("p (j t) -> p j t", j=4)), r=[("ps", 7)], w=[("YTT", n % 2)])
                c = n % 16; qq = n // 16
                c0_ = qq * 256 + (c % 2) * 128
                P.dma("sp", e1_in[c // 2].ap().rearrange("(j p) t -> p j t", p=128)[:, :, c0_:c0_ + 128], ytt[:],
                      r=[("YTT", n % 2)], w=[("e1i", n)], semkey=("YTT", n % 2))

            f_load(0)
            for gi in range(6):
                f_mm(0, gi)
            f_post(0)
            for n in range(NCH):
                nx = n + 1 < NCH
                if nx:
                    f_load(n + 1)
                b_s0(n)
                if nx:
                    f_mm(n + 1, 0); f_mm(n + 1, 1)
                b_s1(n)
                if nx:
                    f_mm(n + 1, 2)
                b_s2(n)
                if nx:
                    f_mm(n + 1, 3); f_mm(n + 1, 4)
                b_s3(n)
                if nx:
                    f_mm(n + 1, 5)
                    f_post(n + 1)
            for c in range(8):
                P.coll(e1_in[c].ap().opt(), e1_out[c].ap().opt(), r=[("e1i", n_) for n_ in range(NCH) if (n_ % 16) // 2 == c],
                       w=[("e1o", c)], semkey="cc")

        def phase_post(L, KM, with_proj, a_out, a_key, hres, hdst, wout):
            pfx[0] = "B%d_" % L; cur[0] = 0
            ln1g = lnp[4 * L + 0:4 * L + 1, :]; ln1b = lnp[4 * L + 1:4 * L + 2, :]
            ln2g = lnp[4 * L + 2:4 * L + 3, :]; ln2b = lnp[4 * L + 3:4 * L + 4, :]
            Y = sb("Y", [128, NTT, 1024], F32)
            HTb = sb("HTb", [128, 8, NT], BF16)
            BIGW = sb("BIGW", [128, 6 * 4096], BF16)
            LG = sb("LG", [128, 1024], F32); LB = sb("LB", [128, 1024], F32)
            RW = sb("RW", [128, 8, 16], F32); RB = sb("RB", [128, 16], F32)
            IDN = sb("IDN", [128, 128], F32)
            SEL = sb("SEL", [16, NE * 128], BF16)
            WT = sb("WT", [16, NT], BF16)
            WPAD = sb("WPAD", [128, 128], F32)
            st6 = sb("st6", [128, 2, 6], F32); mv = sb("mv", [128, 2], F32); rstd = sb("rstd", [128, 1], F32)
            R = {n: sb("r_" + n, [128, 16], F32) for n in ("aff", "ss", "eq", "a2", "ms", "eq1", "ms2", "ch", "w")}
            R4 = {n: sb("r4_" + n, [128, 4], F32) for n in ("m1", "m2", "gs", "ing")}
            R1 = {n: sb("r1_" + n, [128, 1], F32) for n in ("gm", "t1", "t2", "ws", "rws")}
            scr0 = cur[0]
            Asb = [sb("Asb%d" % i, [128, KM, 128], BF16) for i in range(2)]
            HR = [sb("HR%d" % i, [128, 1024], F32) for i in range(2)]
            U = sb("U", [128, 1024], F32)
            HA = sb("HA", [128, 1024], F32)
            HT32 = [sb("HT32_%d" % i, [128, 8, 128], F32) for i in range(2)]
            cur[0] = scr0
            WBC = [sb("WBC%d" % i, [128, 512], BF16) for i in range(2)]
            SG = [sb("SG%d" % i, [128, 512], BF16) for i in range(2)]
            TU = [sb("TU%d" % i, [128, 512], BF16) for i in range(2)]
            ACTV = [sb("ACTV%d" % i, [128, 4, 512], BF16) for i in range(2)]
            if with_proj:
                cur[0] = scr0
                CS = [sb("CS%d" % i, [128, 64], F32) for i in range(2)]
                HB16 = [sb("HB16_%d" % i, [128, 8, 128], BF16) for i in range(2)]
                PR = [sb("PR%d" % i, [128, 2608], F32) for i in range(2)]
                T1 = sb("T1", [128, 256], F32); T2 = sb("T2", [128, 256], F32)
                XTt = [sb("XTt%d" % i, [128, 16, 128], BF16) for i in range(2)]
                VB = [sb("VB%d" % i, [128, 512], BF16) for i in range(1)]
                GTt = [sb("GTt%d" % i, [48, 128], F32) for i in range(1)]

            Wout = BIGW[:, 0:KM * 1024].rearrange("p (k n) -> p k n", k=KM)
            ring = [BIGW[:, s * 4096:(s + 1) * 4096] for s in range(6)]

            P.dve(lambda e: e.memset(WPAD[:], 0.0), w=["w"])
            P.dma("sp", IDN[:], ident_d, w=["IDN"], semkey="IDN")
            P.dma("sp", RW[:], rw.rearrange("(k p) n -> p k n", p=128), w=["RW"], semkey="RW")
            P.dma("sp", RB[:], rbias.partition_broadcast(128), w=["RB"], semkey="RB")
            P.dma("sp", LG[:], ln1g.partition_broadcast(128), w=["LG"], semkey="LG")
            P.dma("sp", LB[:], ln1b.partition_broadcast(128), w=["LB"], semkey="LB")
            P.dma("pool", SEL[:], sel_d, w=["SEL"], semkey="SEL")
            woutv = wout.rearrange("(k p) n -> p k n", p=128)
            for k0 in range(0, KM, 4):
                P.dma("pool", Wout[:, k0:k0 + 4, :], woutv[:, k0:k0 + 4, :], w=[("Wout", k0)], semkey=("Wout", k0))
            WoutKeys = [("Wout", k0) for k0 in range(0, KM, 4)]

            def layer_norm(src_key, src, dst, dst_keys, extra_r=()):
                P.dve(lambda e: e.bn_stats(st6[:, 0, :], src[:, 0:512]), r=[src_key], w=["st6a"])
                P.dve(lambda e: e.bn_stats(st6[:, 1, :], src[:, 512:1024]), r=[src_key], w=["st6b"])
                P.dve(lambda e: e.bn_aggr(mv[:], st6[:].rearrange("p a b -> p (a b)")), r=["st6a", "st6b"], w=["mv"])
                P.dve(lambda e: e.tensor_scalar(rstd[:], mv[:, 1:2], LN_EPS, None, ALU.add), r=["mv", "rstd"], w=["rstd"])
                P.act(lambda e: e.activation(out=rstd[:], in_=rstd[:], func=AF.Sqrt), r=["rstd"], w=["rstd"])
                P.dve(lambda e: e.reciprocal(rstd[:], rstd[:]), r=["rstd"], w=["rstd"])
                P.dve(lambda e: e.tensor_scalar(dst, src, mv[:, 0:1], rstd[:, 0:1], ALU.subtract, ALU.mult),
                      r=[src_key, "mv", "rstd"] + list(extra_r), w=list(dst_keys))
                P.dve(lambda e: e.tensor_tensor(dst, dst, LG[:], ALU.mult), r=list(dst_keys) + ["LG"], w=list(dst_keys))
                P.dve(lambda e: e.tensor_tensor(dst, dst, LB[:], ALU.add), r=list(dst_keys) + ["LB"], w=list(dst_keys))

            def s1_a1(tt):
                ab = tt % 2
                A = Asb[ab]; H = HR[ab]
                P.dma_dyn("sp" if L == 0 else "pool", lambda g, A=A, tt=tt: (A[:], a_out(tt, g)),
                          w=[("A", ab)], semkey=("A", ab))
                P.dma("sp", H[:], hres[tt * 128:(tt + 1) * 128, :], w=[("HR", ab)], semkey=("HR", ab))
                for half in range(2):
                    ps = PS[half]
                    for k in range(KM):
                        P.pe(lambda e, ps=ps, k=k, half=half, A=A: e.matmul(
                            ps[:], A[:, k, :], Wout[:, k, half * 512:(half + 1) * 512], start=(k == 0), stop=(k == KM - 1)),
                            r=[("A", ab), ("Wout", (k // 4) * 4)], w=[("ps", half)])
                    P.dve(lambda e, ps=ps, half=half, H=H: e.scalar_tensor_tensor(
                        U[:, half * 512:(half + 1) * 512], H[:, half * 512:(half + 1) * 512], ALPHA, ps[:], ALU.mult, ALU.add),
                        r=[("ps", half), ("HR", ab)], w=["U"])
                layer_norm("U", U[:], HA[:], ["hA"])
                P.act(lambda e, tt=tt: e.mul(Y[:, tt, :], HA[:], ALPHA), r=["hA"], w=[("Y", tt)])
            def s1_a2(tt):
                ab = tt % 2
                for kc in range(8):
                    pb = 2 + (kc % 2)
                    P.pe(lambda e, kc=kc, pb=pb: e.transpose(PS[pb][:, 0:128], HA[:, kc * 128:(kc + 1) * 128], IDN[:]),
                         r=["hA", "IDN"], w=[("ps", pb)])
                    P.act(lambda e, kc=kc, pb=pb, tt=tt: e.copy(HTb[:, kc, tt * 128:(tt + 1) * 128], PS[pb][:, 0:128]),
                          r=[("ps", pb)], w=[("HTb", tt // 4, kc)])
                    P.dve(lambda e, kc=kc, pb=pb: e.tensor_copy(HT32[tt % 2][:, kc, :], PS[pb][:, 0:128]),
                          r=[("ps", pb)], w=[("HT32", tt % 2, kc)])
            def s1_bm(tt):
                ab = tt % 2
                for kc in range(8):
                    P.pe(lambda e, kc=kc: e.matmul(PS[4][:, 0:16], HT32[tt % 2][:, kc, :], RW[:, kc, :], start=(kc == 0), stop=(kc == 7)),
                         r=[("HT32", tt % 2, kc), "RW"], w=[("ps", 4)])
                r = R; r4 = R4; r1 = R1
                P.act(lambda e: e.activation(out=r["aff"][:], in_=PS[4][:, 0:16], func=AF.Sigmoid), r=[("ps", 4)], w=["aff"])
                P.dve(lambda e: e.tensor_tensor(r["ss"][:], r["aff"][:], RB[:], ALU.add), r=["aff", "RB"], w=["ss"])
                ss3 = r["ss"][:].rearrange("p (g k) -> p g k", g=4)
                P.dve(lambda e: e.tensor_reduce(r4["m1"][:], ss3, AX.X, ALU.max), r=["ss"], w=["m1"])
                P.dve(lambda e: e.tensor_tensor(r["eq"][:].rearrange("p (g k) -> p g k", g=4), ss3,
                                                 r4["m1"][:].unsqueeze(2).broadcast_to([128, 4, 4]), ALU.is_equal),
                      r=["ss", "m1"], w=["eq"])
                P.dve(lambda e: e.scalar_tensor_tensor(r["a2"][:], r["eq"][:], -1.0e9, r["ss"][:], ALU.mult, ALU.add),
                      r=["eq", "ss"], w=["a2"])
                P.dve(lambda e: e.tensor_reduce(r4["m2"][:], r["a2"][:].rearrange("p (g k) -> p g k", g=4), AX.X, ALU.max),
                      r=["a2"], w=["m2"])
                P.dve(lambda e: e.tensor_tensor(r4["gs"][:], r4["m1"][:], r4["m2"][:], ALU.add), r=["m1", "m2"], w=["gs"])
                P.dve(lambda e: e.tensor_reduce(r1["gm"][:], r4["gs"][:], AX.X, ALU.max), r=["gs"], w=["gm"])
                P.dve(lambda e: e.tensor_scalar(r4["ing"][:], r4["gs"][:], r1["gm"][:, 0:1], None, ALU.is_ge), r=["gs", "gm"], w=["ing"])
                P.dve(lambda e: e.tensor_scalar(r4["ing"][:], r4["ing"][:], 1.0, 1.0e9, ALU.subtract, ALU.mult), r=["ing"], w=["ing"])
                P.dve(lambda e: e.tensor_tensor(r["ms"][:].rearrange("p (g k) -> p g k", g=4), ss3,
                                                 r4["ing"][:].unsqueeze(2).broadcast_to([128, 4, 4]), ALU.add),
                      r=["ss", "ing"], w=["ms"])
                P.dve(lambda e: e.tensor_reduce(r1["t1"][:], r["ms"][:], AX.X, ALU.max), r=["ms"], w=["t1"])
                P.dve(lambda e: e.tensor_scalar(r["eq1"][:], r["ms"][:], r1["t1"][:, 0:1], -1.0e9, ALU.is_equal, ALU.mult),
                      r=["ms", "t1"], w=["eq1"])
                P.dve(lambda e: e.tensor_tensor(r["ms2"][:], r["eq1"][:], r["ms"][:], ALU.add), r=["eq1", "ms"], w=["ms2"])
                P.dve(lambda e: e.tensor_reduce(r1["t2"][:], r["ms2"][:], AX.X, ALU.max), r=["ms2"], w=["t2"])
                P.dve(lambda e: e.tensor_scalar(r["ch"][:], r["ms"][:], r1["t2"][:, 0:1], None, ALU.is_ge), r=["ms", "t2"], w=["ch"])
                P.dve(lambda e: e.tensor_tensor(WPAD[:, 0:16], r["ch"][:], r["aff"][:], ALU.mult), r=["ch", "aff"], w=["w"])
                P.dve(lambda e: e.tensor_reduce(r1["ws"][:], WPAD[:, 0:16], AX.X, ALU.add), r=["w"], w=["ws"])
                P.dve(lambda e: e.reciprocal(r1["rws"][:], r1["ws"][:]), r=["ws"], w=["rws"])
                P.dve(lambda e: e.tensor_scalar(WPAD[:, 0:16], WPAD[:, 0:16], r1["rws"][:, 0:1], None, ALU.mult), r=["w", "rws"], w=["w"])
            def s1_bt(tt):
                P.pe(lambda e: e.transpose(PS[5][:, 0:128], WPAD[:], IDN[:]), r=["w", "IDN"], w=[("ps", 5)])
                P.act(lambda e, tt=tt: e.copy(WT[:, tt * 128:(tt + 1) * 128], PS[5][0:16, 0:128]), r=[("ps", 5)], w=[("WT", tt // 4)])
            s1_a1(0)
            s1_a2(0)
            for tt in range(NTT):
                if tt + 1 < NTT:
                    s1_a1(tt + 1)
                s1_bm(tt)
                if tt + 1 < NTT:
                    s1_a2(tt + 1)
                s1_bt(tt)

            P.barrier()
            wgv = wg[L].rearrange("e (k p) n -> e p k n", p=128)
            wuv = wu[L].rearrange("e (k p) n -> e p k n", p=128)
            wdv = wd[L].rearrange("e (k p) n -> e p k n", p=128)
            nload = [0]

            def ring_load(src, shape_k):
                s = nload[0] % 6
                first = nload[0] < 6
                nload[0] += 1
                dst = ring[s].rearrange("p (k n) -> p k n", k=shape_k)
                wk = [("ring", s)] + (WoutKeys if first else [])
                P.dma("pool", dst, src, w=wk, semkey=("ring", s))
                return s, dst

            pend = []

            def prefetch(e):
                pend.append((ring_load(wgv[e], 8), ring_load(wuv[e], 8), ring_load(wdv[e], 4)))
            prefetch(0)
            it = 0
            for e_ in range(NE):
                if e_ + 1 < NE:
                    prefetch(e_ + 1)
                (sg_, G), (su_, Uw), (sd_, Dn) = pend.pop(0)
                for tg in range(4):
                    wb = WBC[it % 2]
                    P.pe(lambda e, e_=e_, tg=tg: e.matmul(PS[6][:], SEL[:, e_ * 128:(e_ + 1) * 128], WT[:, tg * 512:(tg + 1) * 512],
                                                           start=True, stop=True), r=["SEL", ("WT", tg)], w=[("ps", 6)])
                    P.act(lambda e, wb=wb: e.copy(wb[:], PS[6][:]), r=[("ps", 6)], w=[("WBC", it % 2)])
                    AV = ACTV[it % 2]
                    for hc in range(4):
                        pg = PS[0 + (hc % 2)]; pu = PS[2 + (hc % 2)]
                        for kc in range(8):
                            P.pe(lambda e, pg=pg, kc=kc, hc=hc, tg=tg, G=G: e.matmul(
                                pg[:], G[:, kc, hc * 128:(hc + 1) * 128], HTb[:, kc, tg * 512:(tg + 1) * 512],
                                start=(kc == 0), stop=(kc == 7)), r=[("ring", sg_), ("HTb", tg, kc)], w=[("ps", 0 + hc % 2)])
                        for kc in range(8):
                            P.pe(lambda e, pu=pu, kc=kc, hc=hc, tg=tg, Uw=Uw: e.matmul(
                                pu[:], Uw[:, kc, hc * 128:(hc + 1) * 128], HTb[:, kc, tg * 512:(tg + 1) * 512],
                                start=(kc == 0), stop=(kc == 7)), r=[("ring", su_), ("HTb", tg, kc)], w=[("ps", 2 + hc % 2)])
                        sgt = SG[hc % 2]; tut = TU[hc % 2]
                        P.act(lambda e, sgt=sgt, pg=pg: e.activation(out=sgt[:], in_=pg[:], func=AF.Silu),
                              r=[("ps", 0 + hc % 2)], w=[("SG", hc % 2)])
                        P.dve(lambda e, tut=tut, pu=pu, sgt=sgt: e.tensor_tensor(tut[:], pu[:], sgt[:], ALU.mult),
                              r=[("ps", 2 + hc % 2), ("SG", hc % 2)], w=[("TU", hc % 2)])
                        P.dve(lambda e, AV=AV, hc=hc, tut=tut, wb=wb: e.tensor_tensor(AV[:, hc, :], tut[:], wb[:], ALU.mult),
                              r=[("TU", hc % 2), ("WBC", it % 2)], w=[("AV", it % 2, hc)])
                    for tl in range(4):
                        tt = tg * 4 + tl
                        for half in range(2):
                            py = PS[4 + half]
                            for hc in range(4):
                                P.pe(lambda e, py=py, AV=AV, hc=hc, tl=tl, half=half, Dn=Dn: e.matmul(
                                    py[:], AV[:, hc, tl * 128:(tl + 1) * 128], Dn[:, hc, half * 512:(half + 1) * 512],
                                    start=(hc == 0), stop=(hc == 3)), r=[("AV", it % 2, hc), ("ring", sd_)], w=[("ps", 4 + half)])
                            P.dve(lambda e, py=py, tt=tt, half=half: e.tensor_tensor(
                                Y[:, tt, half * 512:(half + 1) * 512], Y[:, tt, half * 512:(half + 1) * 512], py[:], ALU.add),
                                r=[("ps", 4 + half), ("Y", tt)], w=[("Y", tt)])
                    it += 1

            P.barrier()
            P.dma("sp", LG[:], ln2g.partition_broadcast(128), w=["LG"], semkey="LG")
            P.dma("sp", LB[:], ln2b.partition_broadcast(128), w=["LB"], semkey="LB")
            if with_proj:
                WKV = BIGW[:, 0:8 * 1536].rearrange("p (k n) -> p k n", k=8)
                WIN = HTb[:].rearrange("p k t -> p (k t)")[:, 0:8 * 1072].rearrange("p (k n) -> p k n", k=8)
                allring = [("ring", s) for s in range(6)] + WoutKeys
                wpv = wp.rearrange("(k p) n -> p k n", p=128)
                P.dma("pool", WKV, wpv[:, :, 0:1536], w=allring + ["WKV"], semkey="WKV")
                allht = [("HTb", tg, kc) for tg in range(4) for kc in range(8)]
                P.dma("pool", WIN, wpv[:, :, 1536:2608], w=allht + ["WIN"], semkey="WIN")
            def s3_ln(tt):
                ob = tt % 2
                layer_norm(("Y", tt), Y[:, tt, :], Y[:, tt, :], [("Y", tt)])
                P.dma("sp", hdst[tt * 128:(tt + 1) * 128, :], Y[:, tt, :], r=[("Y", tt)], w=[("hdst", tt)], semkey=("hout", tt % 4))
                if with_proj:
                    P.dma("sp", CS[ob][:], cs_d[tt * 128:(tt + 1) * 128, :], w=[("CS", ob)], semkey=("CS", ob))

            def s3_tr(tt):
                ob = tt % 2
                hb = HB16[ob]
                for kc in range(8):
                    pb = 6 + (kc % 2)
                    P.pe(lambda e, kc=kc, pb=pb, tt=tt: e.transpose(PS[pb][:, 0:128], Y[:, tt, kc * 128:(kc + 1) * 128], IDN[:]),
                         r=[("Y", tt), "IDN"], w=[("ps", pb)])
                    P.act(lambda e, kc=kc, pb=pb, hb=hb: e.copy(hb[:, kc, :], PS[pb][:, 0:128]), r=[("ps", pb)], w=[("HB16", ob, kc)])

            PRK = {}

            def s3_grp(tt):
                ob = tt % 2
                hb = HB16[ob]; cs = CS[ob]; pr = PR[ob]
                groups = [(WKV, 0, 512, 0, "WKV"), (WKV, 512, 512, 512, "WKV"), (WKV, 1024, 512, 1024, "WKV"),
                          (WIN, 0, 512, 1536, "WIN"), (WIN, 512, 512, 2048, "WIN"), (WIN, 1024, 48, 2560, "WIN")]
                prk = []
                for gi, (Wv, c0, ncol, oc, wkey) in enumerate(groups):
                    pb = gi % 6
                    ps = PS[pb]
                    for kc in range(8):
                        P.pe(lambda e, ps=ps, kc=kc, Wv=Wv, c0=c0, ncol=ncol, hb=hb: e.matmul(
                            ps[:, 0:ncol], hb[:, kc, :], Wv[:, kc, c0:c0 + ncol], start=(kc == 0), stop=(kc == 7)),
                            r=[("HB16", ob, kc), wkey], w=[("ps", pb)])
                    if gi == 5:
                        P.act(lambda e, ps=ps, pr=pr: e.activation(out=pr[:, 2560:2608], in_=ps[:, 0:48], func=AF.Sigmoid),
                              r=[("ps", pb)], w=[("PR", ob, gi)])
                        prk.append(("PR", ob, gi))
                        continue
                    for sbk in range(2):
                        part = (c0 + sbk * 256) // 256 if gi < 3 else None
                        roped = (gi >= 3) or (part in (0, 2, 4))
                        src = ps[:, sbk * 256:(sbk + 1) * 256]
                        dst = pr[:, oc + sbk * 256: oc + (sbk + 1) * 256]
                        kk = ("PR", ob, gi, sbk)
                        if not roped:
                            P.act(lambda e, src=src, dst=dst: e.copy(dst, src), r=[("ps", pb)], w=[kk])
                            prk.append(kk)
                            continue
                        s4 = src.rearrange("p (h two j) -> p h two j", h=4, two=2)
                        d4 = dst.rearrange("p (h two j) -> p h two j", h=4, two=2)
                        x1 = s4[:, :, 0, :]; x2 = s4[:, :, 1, :]
                        cosb = cs[:, 0:32].unsqueeze(1).broadcast_to([128, 4, 32])
                        sinb = cs[:, 32:64].unsqueeze(1).broadcast_to([128, 4, 32])
                        t1 = T1[:, 0:128].rearrange("p (h j) -> p h j", h=4); t2 = T1[:, 128:256].rearrange("p (h j) -> p h j", h=4)
                        t3 = T2[:, 0:128].rearrange("p (h j) -> p h j", h=4); t4 = T2[:, 128:256].rearrange("p (h j) -> p h j", h=4)
                        P.dve(lambda e, t1=t1, x1=x1, cosb=cosb: e.tensor_tensor(t1, x1, cosb, ALU.mult), r=[("ps", pb), ("CS", ob)], w=["T1a"])
                        P.dve(lambda e, t2=t2, x2=x2, sinb=sinb: e.tensor_tensor(t2, x2, sinb, ALU.mult), r=[("ps", pb), ("CS", ob)], w=["T1b"])
                        P.dve(lambda e, t3=t3, x2=x2, cosb=cosb: e.tensor_tensor(t3, x2, cosb, ALU.mult), r=[("ps", pb), ("CS", ob)], w=["T2a"])
                        P.dve(lambda e, t4=t4, x1=x1, sinb=sinb: e.tensor_tensor(t4, x1, sinb, ALU.mult), r=[("ps", pb), ("CS", ob)], w=["T2b"])
                        P.dve(lambda e, d4=d4, t1=t1, t2=t2: e.tensor_tensor(d4[:, :, 0, :], t1, t2, ALU.subtract), r=["T1a", "T1b"], w=[kk + (0,)])
                        P.dve(lambda e, d4=d4, t3=t3, t4=t4: e.tensor_tensor(d4[:, :, 1, :], t3, t4, ALU.add), r=["T2a", "T2b"], w=[kk + (1,)])
                        prk += [kk + (0,), kk + (1,)]
                PRK[tt] = prk

            def s3_x(tt):
                ob = tt % 2
                pr = PR[ob]; prk = PRK[tt]
                xt = XTt[ob]; vb = VB[0]; gt = GTt[0]
                th = tt // 8; tl = tt % 8
                for idx in range(16):
                    ty, gp = idx // 2, idx % 2
                    b_, s_ = idx // 4, idx % 4
                    c0 = TYCOL[ty] + gp * 128
                    P.pe(lambda e, b_=b_, s_=s_, c0=c0, pr=pr: e.transpose(PS[b_][:, s_ * 128:(s_ + 1) * 128], pr[:, c0:c0 + 128], IDN[:]),
                         r=prk + ["IDN"], w=[("ps", b_)])
                    if s_ == 3:
                        if b_ % 2 == 0:
                            P.act(lambda e, b_=b_, xt=xt: e.copy(xt[:, 4 * b_:4 * b_ + 4, :], PS[b_][:].rearrange("p (s t) -> p s t", s=4)),
                                  r=[("ps", b_)], w=[("XTt", ob, b_)])
                        else:
                            P.dve(lambda e, b_=b_, xt=xt: e.tensor_copy(xt[:, 4 * b_:4 * b_ + 4, :], PS[b_][:].rearrange("p (s t) -> p s t", s=4)),
                                  r=[("ps", b_)], w=[("XTt", ob, b_)])
                for ty in range(8):
                    P.dma("sp", e2x_in[ty][th].ap().rearrange("(gp p) t -> p gp t", p=128)[:, :, tl * 128:(tl + 1) * 128],
                          xt[:, 2 * ty:2 * ty + 2, :], r=[("XTt", ob, ty // 2)], w=[("e2xi", ty, tt)], semkey=("XTst", ob, ty // 2))
                P.pe(lambda e, pr=pr: e.transpose(PS[4][0:48, 0:128], pr[:, 2560:2608], IDN[:]), r=prk + ["IDN"], w=[("ps", 4)])
                P.act(lambda e, gt=gt: e.copy(gt[:], PS[4][0:48, 0:128]), r=[("ps", 4)], w=["GTt"])
                P.dma("sp", e2g_in.ap()[:, tt * 128:(tt + 1) * 128], gt[:], r=["GTt"], w=[("e2gi", tt)], semkey="GTst")
                P.act(lambda e, vb=vb, pr=pr: e.copy(vb[:, 0:256], pr[:, 768:1024]), r=prk, w=[("VB", 0)])
                P.act(lambda e, vb=vb, pr=pr: e.copy(vb[:, 256:512], pr[:, 1280:1536]), r=prk, w=[("VB", 1)])
                P.dma("sp", e2v_in[tt // 4].ap()[(tt % 4) * 128:(tt % 4 + 1) * 128, :], vb[:], r=[("VB", 0), ("VB", 1)],
                      w=[("e2vi", tt)], semkey="VBst")
                if tt in (7, 15):
                    th_ = tt // 8
                    for ty in range(8):
                        P.coll(e2x_in[ty][th_].ap().opt(), e2x_out[ty][th_].ap().opt(), r=[("e2xi", ty, th_ * 8 + i) for i in range(8)],
                               w=[("e2xo", ty, th_)], semkey="cc")
                    for v in (2 * th_, 2 * th_ + 1):
                        P.coll(e2v_in[v].ap().opt(), e2v_out[v].ap().opt(), r=[("e2vi", v * 4 + i) for i in range(4)], w=[("e2vo", v)], semkey="cc")

            if not with_proj:
                for tt in range(NTT):
                    s3_ln(tt)
            else:
                s3_ln(0)
                s3_tr(0)
                for tt in range(NTT):
                    if tt + 1 < NTT:
                        s3_ln(tt + 1)
                    s3_grp(tt)
                    if tt + 1 < NTT:
                        s3_tr(tt + 1)
                    s3_x(tt)
            if with_proj:
                P.coll(e2g_in.ap().opt(), e2g_out.ap().opt(), r=[("e2gi", i) for i in range(16)], w=["e2go"], semkey="cc")

        def phase_attn():
            pfx[0] = "C_"; cur[0] = 0
            KA = sb("KA", [128, 64 * 128], BF16)
            KW = sb("KW", [64, S], BF16)
            VS = sb("VS", [128, 64, 65], BF16); VW = sb("VW", [128, 64, 65], BF16)
            KCM = sb("KCM", [64, 512], BF16); VCM = sb("VCM", [128, 4, 65], BF16)
            OVb = sb("OVb", [128, 4, 128], BF16); CMK = sb("CMK", [128, 16, 2, 128], BF16)
            CAUS = sb("CAUS", [128, 512], BF16); ANTI = sb("ANTI", [128, 512], BF16)
            IDb = sb("IDb", [128, 128], BF16); ID32 = sb("ID32", [128, 128], F32)
            BT = sb("BT", [128, 256], F32)
            ONE32 = sb("ONE32", [128, 64], F32); ONEb = sb("ONEb", [128, 64], BF16)
            RA = [[sb("RA%d%d" % (p, h), [128, 512], BF16) for h in range(2)] for p in range(2)]
            GT = [sb("GT%d" % p, [65, 3, 512], F32) for p in range(2)]
            PC = [sb("PC%d" % i, [128, 512], BF16) for i in range(4)]
            PT = [sb("PT%d" % i, [128, 512], BF16) for i in range(4)]
            DENp = [sb("DEN%d" % p_, [65, 3, 512], F32) for p_ in range(2)]; FACb = sb("FACb", [65, 3, 512], BF16)
            OC = [sb("OC%d" % p_, [64, 512], F32) for p_ in range(3)]
            O3 = [sb("O3_%d" % p_, [65, 512], F32) for p_ in range(2)]; O4 = [sb("O4_%d" % p_, [65, 512], F32) for p_ in range(2)]
            RS = sb("RS", [128, 4], F32); IMPs = sb("IMPs", [128, 128], F32); SC = sb("SC", [128, 128], F32)
            M8 = sb("M8", [128, 16], F32); TMP = sb("TMP", [128, 128], F32)
            SELB = sb("SELB", [128, 128], F32); SB2 = sb("SB2", [128, 128], F32)
            BC = [sb("BC%d" % j, [64, 512], F32) for j in range(3)]
            OACC = [sb("OACC%d" % p, [64, 512], F32) for p in range(2)]
            TMPo = sb("TMPo", [64, 512], F32)
            KC2 = sb("KC2", [128, S + 32], BF16)
            W1 = sb("W1", [128, 16, 256], BF16); W2 = sb("W2", [128, 2, 64], BF16); PE = sb("PE", [128, 16], BF16)
            BIAS = sb("BIAS", [128, 2], F32); HID = sb("HID", [128, 2, 512], BF16)

            ch = lambda ap, b: ap.rearrange("p (a b) -> p a b", b=b)

            def xsrc(ty, th, g):
                return e2x_out[ty][th].ap().rearrange("(k f) t -> f k t", k=4)[ds(g * 64, 64), :, :]

            def kdst(T_, p0, th):
                return T_[p0:p0 + 64, 0:S].rearrange("p (k h t) -> p k h t", k=4, h=2)[:, :, th, :]

            n_ = 0
            for ty in range(8):
                for th in range(2):
                    P.dma_dyn("pool" if n_ % 2 == 0 else "sp",
                              lambda g, ty=ty, th=th: (locx[ty][th].ap().rearrange("d (k t) -> d k t", k=4), xsrc(ty, th, g)),
                              w=[("lx", ty, th)], semkey=("lx", n_ % 4))
                    n_ += 1
            P.dma_dyn("sp", lambda g: (locg.ap().rearrange("(o k rj) t -> o k (rj t)", o=1, k=4),
                                       e2g_out.ap().rearrange("(k g rj) t -> g k (rj t)", k=4, g=4)[ds(g, 1), :, :]),
                      w=["lg"], semkey="lg")

            def lsrc(ty, th):
                return locx[ty][th].ap().rearrange("d (k t) -> d k t", k=4)
            for th in range(2):
                P.dma("pool", kdst(KA, 0, th), lsrc(2, th), r=[("lx", 2, th)], w=[("KAk", th)], semkey="KAk")
                P.dma("sp", kdst(KW, 0, th), lsrc(3, th), r=[("lx", 3, th)], w=[("KW", th)], semkey="KW")
            KAk = [("KAk", 0), ("KAk", 1)]; KWk = [("KW", 0), ("KW", 1)]
            for hh in range(2):
                P.dma("pool", ch(KA[64:128, hh * 4096:(hh + 1) * 4096], 2048), ch(ind_d, 2048), w=[("KAi", hh)], semkey=("KAi", hh))
            P.dve(lambda e: e.memset(VS[:], 1.0), w=["VS"])
            P.dve(lambda e: e.memset(VW[:], 1.0), w=["VW"])
            P.dve(lambda e: e.memset(VCM[:], 1.0), w=["VCM"])
            P.dve(lambda e: e.memset(ONE32[:], 1.0), w=["ONE32"])
            P.dve(lambda e: e.memset(ONEb[:], 1.0), w=["ONEb"])
            P.dve(lambda e: e.memset(SB2[:], 0.0), w=["SB2"])
            P.dve(lambda e: e.memset(KC2[0:64, S:S + 32], 0.0), w=["KC2z0"])
            P.dve(lambda e: e.memset(KC2[64:128, S - 1:S + 32], 0.0), w=["KC2z1"])
            for v in range(4):
                for k in range(4):
                    for (T_, name, c0) in ((VS, "VS", 0), (VW, "VW", 256)):
                        def f(g, T_=T_, v=v, c0=c0, k=k):
                            src = e2v_out[v].ap().rearrange("(k kk p) c -> p k kk c", k=4, kk=4)[:, k, :, ds(c0 + g * 64, 64)]
                            kt0 = k * 16 + v * 4
                            return T_[:, kt0:kt0 + 4, 0:64], src
                        P.dma_dyn("sp" if k % 2 == 0 else "pool", f, r=[name], w=[name], semkey=(name, k % 2))
            P.dma("pool", OVb[:], ov_d.rearrange("(k p) n -> p k n", p=128), w=["OVb"], semkey="OVb")
            P.dma("pool", ch(CMK[:].rearrange("p a b c -> p (a b c)"), 2048), ch(cmk_d, 2048), w=["CMK"], semkey="CMK")
            P.dma("pool", CAUS[:], caus_d, w=["CAUS"], semkey="CAUS")
            P.dma("pool", ANTI[:], anti_d, w=["ANTI"], semkey="ANTI")
            P.dma("pool", IDb[:], ident_d, w=["IDb"], semkey="IDb")
            P.dma("sp", ID32[:], ident_d, w=["ID32"], semkey="ID32")
            P.dma("sp", BT[:], bt_d, w=["BT"], semkey="BT")

            for which in range(2):
                w1 = w1k if which == 0 else w1v
                w2 = w2k if which == 0 else w2v
                pe = pek if which == 0 else pev
                kc2keys = []
                for th in range(2):
                    P.dma("sp", kdst(KC2, 0, th), lsrc(which, th), r=["KC2z0", "KC2z1", ("lx", which, th)],
                          w=[("KC2", 0, th)], semkey=("KC2", 0))
                    kc2keys.append(("KC2", 0, th))
                for k in range(4):
                    for th in range(2):
                        c0 = k * 2048 + th * 1024 - 1
                        t0 = 0
                        if c0 < 0:
                            c0 = 0; t0 = 1
                        P.dma("pool", KC2[64:128, c0:c0 + 1024 - t0], locx[which][th].ap()[:, k * 1024 + t0:(k + 1) * 1024],
                              r=["KC2z0", "KC2z1", ("lx", which, th)], w=[("KC2", 1, k, th)], semkey=("KC2", 1))
                        kc2keys.append(("KC2", 1, k, th))
                P.dma("pool", W1[:], w1.rearrange("(j p) n -> p j n", p=128), w=["W1"], semkey="W1")
                P.dma("pool", W2[:], w2.rearrange("(k p) n -> p k n", p=128), w=["W2"], semkey="W2")
                P.dma("pool", PE[:], pe, w=["PE"], semkey="PE")
                for hcn in range(2):
                    for j in range(16):
                        P.pe(lambda e, hcn=hcn, j=j: e.matmul(PS[7][:, hcn:hcn + 1], W1[:, j, hcn * 128:(hcn + 1) * 128], PE[:, j:j + 1],
                                                              start=(j == 0), stop=(j == 15)), r=["W1", "PE"], w=[("ps", 7)])
                P.dve(lambda e: e.tensor_copy(BIAS[:], PS[7][:, 0:2]), r=[("ps", 7)], w=["BIAS"])
                for hcn in range(2):
                    for j in range(16):
                        rhs = KC2[:, 2 * j: 2 * j + 16 * 512].rearrange("p (n s) -> p n s", s=16)[:, :, 0]
                        P.pe(lambda e, hcn=hcn, j=j, rhs=rhs: e.matmul(PS[hcn][:], W1[:, j, hcn * 128:(hcn + 1) * 128], rhs,
                                                                        start=(j == 0), stop=(j == 15)),
                             r=["W1", "KC2z0", "KC2z1"] + kc2keys, w=[("ps", hcn)])
                    P.act(lambda e, hcn=hcn: e.activation(out=HID[:, hcn, :], in_=PS[hcn][:], func=AF.Gelu_apprx_tanh, bias=BIAS[:, hcn:hcn + 1]),
                          r=[("ps", hcn), "BIAS"], w=[("HID", hcn)])
                if which == 0:
                    for hcn in range(2):
                        P.pe(lambda e, hcn=hcn: e.matmul(PS[2][0:64, :], W2[:, hcn, :], HID[:, hcn, :], start=(hcn == 0), stop=(hcn == 1)),
                             r=["W2", ("HID", hcn)], w=[("ps", 2)])
                    P.act(lambda e: e.copy(KCM[:], PS[2][0:64, :]), r=[("ps", 2)], w=["KCM"])
                else:
                    for nt in range(4):
                        for hcn in range(2):
                            P.pe(lambda e, hcn=hcn, nt=nt: e.matmul(PS[3][:, nt * 64:(nt + 1) * 64], HID[:, hcn, nt * 128:(nt + 1) * 128], W2[:, hcn, :],
                                                                     start=(hcn == 0), stop=(hcn == 1)), r=["W2", ("HID", hcn)], w=[("ps", 3)])
                    P.act(lambda e: e.copy(VCM[:, :, 0:64], PS[3][:, 0:256].rearrange("p (a b) -> p a b", a=4)), r=[("ps", 3), "VCM"], w=["VCM"])
            P.barrier()

            sctr = [0]

            def score_tile(mm_list, dst, dst_key, extra_r):
                b = (0, 1, 6)[sctr[0] % 3]
                sctr[0] += 1
                n = len(mm_list)
                for ii, (l, r_, ks) in enumerate(mm_list):
                    P.pe(lambda e, l=l, r_=r_, ii=ii, b=b: e.matmul(PS[b][:], l, r_, start=(ii == 0), stop=(ii == n - 1)),
                         r=ks, w=[("ps", b)])
                P.act(lambda e, b=b, dst=dst: e.activation(out=dst[:], in_=PS[b][:], func=AF.Exp, scale=0.125),
                      r=[("ps", b)] + list(extra_r), w=[dst_key])

            ptc = [0]

            def fa(i, par, oc):
                nh = 2 if i >= 32 else 1
                kr = i // 16; th = (i % 16) // 8; t0 = ((i % 16) % 8) * 128
                DENi = DENp[par]
                for h in range(nh):
                    for r in range(4):
                        P.dma("pool" if r % 2 == 0 else "sp", RA[par][h][0:64, r * 128:(r + 1) * 128],
                              locx[4 + r][th].ap()[:, kr * 1024 + t0:kr * 1024 + t0 + 128],
                              r=[("lx", 4 + r, th)], w=[("RAq", par, h, r)], semkey=("RAq", par, h, r % 2))
                P.dma("sp", GT[par][64:65, :, :].rearrange("p j (r q) -> p j r q", r=4),
                      locg.ap().rearrange("(k r j) t -> k j r t", k=4, r=4)[kr:kr + 1, :, :, (i % 16) * 128:(i % 16 + 1) * 128],
                      r=["lg"], w=[("GT", par)], semkey=("GT", par))
                QA = RA[par][0][0:64, :]
                qk = [("RAq", par, 0, r) for r in range(4)]
                ctl = (8 * i + 6) // 128
                nct = ctl + 1
                pat = i % 16
                for ct in range(nct):
                    score_tile([(KCM[:, ct * 128:(ct + 1) * 128], QA, ["KCM"] + qk)], PC[ct], ("PC", ct), [])
                    if ct == ctl or (ct == ctl - 1 and pat == 0):
                        wh = 0 if ct == ctl else 1
                        P.dve(lambda e, ct=ct, wh=wh: e.tensor_tensor(PC[ct][:].rearrange("p (r q) -> p r q", r=4),
                                                                       PC[ct][:].rearrange("p (r q) -> p r q", r=4),
                                                                       CMK[:, pat, wh, :].unsqueeze(1).broadcast_to([128, 4, 128]), ALU.mult),
                              r=[("PC", ct), "CMK"], w=[("PC", ct)])

            def fa2(i, par, oc):
                DENi = DENp[par]
                nct = (8 * i + 6) // 128 + 1
                for ct in range(nct):
                    P.pe(lambda e, ct=ct: e.matmul(PS[2][0:65, :], VCM[:, ct, :], PC[ct][:], start=(ct == 0), stop=(ct == nct - 1)),
                         r=["VCM", ("PC", ct)], w=[("ps", 2)])
                for r in range(4):
                    for ct in range(nct):
                        P.pe(lambda e, ct=ct, r=r: e.matmul(PS[5][:, r * 128:(r + 1) * 128], PC[ct][:, r * 128:(r + 1) * 128], OVb[:, ct, :],
                                                            start=(ct == 0), stop=(ct == nct - 1)), r=[("PC", ct), "OVb"], w=[("ps", 5)])
                P.dve(lambda e: e.tensor_scalar(DENi[64:65, 0, :], PS[2][64:65, :], 1e-30, None, ALU.max), r=[("ps", 2)], w=[("DEN", par, 0)])
                P.act(lambda e: e.copy(OC[oc][:], PS[2][0:64, :]), r=[("ps", 2)], w=[("OC", oc)])

            def fb(i, par):
                DENi = DENp[par]
                for r in range(4):
                    P.pe(lambda e, r=r: e.matmul(PS[7][:, r:r + 1], DENi[64:65, 0, r * 128:(r + 1) * 128], ONE32[64:65, 0:1], start=True, stop=True),
                         r=[("DEN", par, 0), "ONE32"], w=[("ps", 7)])
                P.dve(lambda e: e.reciprocal(RS[:], PS[7][:, 0:4]), r=[("ps", 7)], w=["RS"])
                P.dve(lambda e: e.tensor_scalar(IMPs[:], PS[5][:, 0:128], RS[:, 0:1], None, ALU.mult), r=[("ps", 5), "RS"], w=["IMPs"])
                for r in range(1, 4):
                    P.dve(lambda e, r=r: e.scalar_tensor_tensor(IMPs[:], PS[5][:, r * 128:(r + 1) * 128], RS[:, r:r + 1], IMPs[:], ALU.mult, ALU.add),
                          r=[("ps", 5), "RS", "IMPs"], w=["IMPs"])
                P.dve(lambda e: e.tensor_tensor(SC[:], IMPs[:], BT[:, 128 - 2 * i:256 - 2 * i], ALU.add), r=["IMPs", "BT"], w=["SC"])
                if i >= 1:
                    P.dve(lambda e: e.tensor_scalar(SC[:, 0:1], SC[:, 0:1], 1000.0, None, ALU.add), r=["SC"], w=["SC"])
                P.dve(lambda e: e.max(M8[:, 0:8], SC[:]), r=["SC"], w=["M8a"])
                P.dve(lambda e: e.match_replace(TMP[:], M8[:, 0:8], SC[:], -3.0e38), r=["SC", "M8a"], w=["TMP"])
                P.dve(lambda e: e.max(M8[:, 8:16], TMP[:]), r=["TMP"], w=["M8b"])
                P.dve(lambda e: e.tensor_scalar(SELB[:], SC[:], M8[:, 15:16], NEGB, ALU.is_lt, ALU.mult), r=["SC", "M8b"], w=["SELB"])
                P.dve(lambda e: e.tensor_copy(SB2[:, 64:128], SELB[:, 0:64]), r=["SELB"], w=["SB2"])

            def fc(i, par):
                nh = 2 if i >= 32 else 1
                P.pe(lambda e: e.transpose(PS[7][:, 128:256], SB2[:], ID32[:]), r=["SB2", "ID32"], w=[("ps", 7)])
                P.act(lambda e: e.copy(RA[par][0][64:128, :].rearrange("p (r q) -> p r q", r=4),
                                       PS[7][64:128, 128:256].unsqueeze(1).broadcast_to([64, 4, 128])), r=[("ps", 7)], w=[("RAs", par, 0)])
                if nh == 2:
                    P.pe(lambda e: e.transpose(PS[7][:, 256:384], SELB[:], ID32[:]), r=["SELB", "ID32"], w=[("ps", 7)])
                    P.act(lambda e: e.copy(RA[par][1][64:128, :].rearrange("p (r q) -> p r q", r=4),
                                           PS[7][64:128, 256:384].unsqueeze(1).broadcast_to([64, 4, 128])), r=[("ps", 7)], w=[("RAs", par, 1)])

            def comb_a(i, par):
                DENi = DENp[par]
                P.dve(lambda e: e.tensor_scalar(DENi[64:65, 1, :], O3[par][64:65, :], 1e-30, None, ALU.max), r=[("O3", par)], w=[("DEN", par, 1)])
                P.dve(lambda e: e.tensor_scalar(DENi[64:65, 2, :], O4[par][64:65, :], 1e-30, None, ALU.max), r=[("O4", par)], w=[("DEN", par, 2)])
                dk = [("DEN", par, 0), ("DEN", par, 1), ("DEN", par, 2)]
                P.dve(lambda e: e.reciprocal(DENi[64:65, :, :], DENi[64:65, :, :]), r=dk, w=dk)
                P.dve(lambda e: e.tensor_tensor(FACb[64:65, :, :], DENi[64:65, :, :], GT[par][64:65, :, :], ALU.mult),
                      r=dk + [("GT", par)], w=["FACb"])

            def comb_b(i, par, oc):
                oa = OACC[par]
                for j in range(3):
                    P.pe(lambda e, j=j: e.matmul(PS[7][0:64, :], ONEb[64:65, 0:64], FACb[64:65, j, :], start=True, stop=True),
                         r=["ONEb", "FACb"], w=[("ps", 7)])
                    if j == 0:
                        P.dve(lambda e, oa=oa: e.tensor_tensor(oa[:], PS[7][0:64, :], OC[oc][:], ALU.mult), r=[("OC", oc), ("ps", 7)], w=[("OACC", par)])
                    else:
                        src = (O3 if j == 1 else O4)[par][0:64, :]
                        skey = ("O3" if j == 1 else "O4", par)
                        P.dve(lambda e, src=src, j=j: e.tensor_tensor(TMPo[:], PS[7][0:64, :], src, ALU.mult), r=[skey, ("ps", 7)], w=["TMPo"])
                        P.dve(lambda e, oa=oa: e.tensor_tensor(oa[:], oa[:], TMPo[:], ALU.add), r=[("OACC", par), "TMPo"], w=[("OACC", par)])
                c = i % 16; qq = i // 16
                P.dma("pool", e3_in[c].ap().rearrange("(r d) t -> d r t", d=64)[:, :, qq * 128:(qq + 1) * 128],
                      oa[:].rearrange("p (r q) -> p r q", r=4), r=[("OACC", par)], w=[("e3i", i)], semkey=("OACC", par))

            def make_tiles(i, par):
                QA = RA[par][0][0:64, :]
                qk = [("RAq", par, 0, r) for r in range(4)]
                tiles = []
                k0 = max(0, i - 4)
                for kt in range(k0, i + 1):
                    mm = [(KW[:, kt * 128:(kt + 1) * 128], QA, KWk + qk)]
                    if kt == i:
                        mm.append((IDb[:], CAUS[:], ["IDb", "CAUS"]))
                    elif kt == i - 4:
                        mm.append((IDb[:], ANTI[:], ["IDb", "ANTI"]))
                    tiles.append((mm, 4, VW, "VW", kt, kt == k0, kt == i))
                for kt in range(i + 1):
                    h = kt // 32
                    mm = [(KA[:, kt * 128:(kt + 1) * 128], RA[par][h][:],
                           KAk + [("KAi", h), ("RAs", par, h)] + [("RAq", par, h, r) for r in range(4)])]
                    if kt == i:
                        mm.append((IDb[:], CAUS[:], ["IDb", "CAUS"]))
                    tiles.append((mm, 3, VS, "VS", kt, kt == 0, kt == i))
                return tiles

            order = [qq_ * 16 + c_ for c_ in range(16) for qq_ in range(4)]
            fa(order[0], 0, 0); fa2(order[0], 0, 0); fb(order[0], 0); fc(order[0], 0)
            for n_ in range(NQB):
                i = order[n_]; par = n_ % 2
                if n_ >= 1:
                    comb_a(order[n_ - 1], (n_ - 1) % 2)
                if n_ + 1 < NQB:
                    fa(order[n_ + 1], (n_ + 1) % 2, (n_ + 1) % 3)
                tiles = make_tiles(i, par)
                T_ = len(tiles)
                pbs = {}

                def emit_score(j):
                    pb = ptc[0] % 4
                    ptc[0] += 1
                    pbs[j] = pb
                    score_tile(tiles[j][0], PT[pb], ("PT", pb), [])

                def emit_pv(j):
                    mm, bank, Vt, vkey, kt, st, sp_ = tiles[j]
                    pb = pbs[j]
                    P.pe(lambda e, kt=kt, pb=pb, bank=bank, Vt=Vt, st=st, sp_=sp_: e.matmul(PS[bank][0:65, :], Vt[:, kt, :], PT[pb][:], start=st, stop=sp_),
                         r=[vkey, ("PT", pb)], w=[("ps", bank)])
                emit_score(0)
                emit_score(1)
                for j in range(T_):
                    if j + 2 < T_:
                        emit_score(j + 2)
                    emit_pv(j)
                    if j == min(1, T_ - 1) and n_ >= 1:
                        comb_b(order[n_ - 1], (n_ - 1) % 2, (n_ - 1) % 3)
                        if (n_ - 1) % 4 == 3:
                            c = (n_ - 1) // 4
                            P.coll(e3_in[c].ap().opt(), e3_out[c].ap().opt(), r=[("e3i", c + 16 * q_) for q_ in range(4)], w=[("e3o", c)], semkey="cc")
                    if n_ + 1 < NQB:
                        p_a2 = min(2, T_ - 1); p_b = max(T_ // 3, p_a2); p_c = max((2 * T_) // 3, p_b)
                        if j == p_a2:
                            fa2(order[n_ + 1], (n_ + 1) % 2, (n_ + 1) % 3)
                        if j == p_b:
                            fb(order[n_ + 1], (n_ + 1) % 2)
                        if j == p_c:
                            fc(order[n_ + 1], (n_ + 1) % 2)
                P.act(lambda e, par=par: e.copy(O3[par][:], PS[3][0:65, :]), r=[("ps", 3)], w=[("O3", par)])
                P.dve(lambda e, par=par: e.tensor_copy(O4[par][:], PS[4][0:65, :]), r=[("ps", 4)], w=[("O4", par)])
            comb_a(order[NQB - 1], (NQB - 1) % 2)
            comb_b(order[NQB - 1], (NQB - 1) % 2, (NQB - 1) % 3)
            P.coll(e3_in[15].ap().opt(), e3_out[15].ap().opt(), r=[("e3i", 15 + 16 * q_) for q_ in range(4)], w=[("e3o", 15)], semkey="cc")

        phase_ret()
        if STOP != "A":
            P.new_phase()
            phase_post(0, 16, True,
                   lambda tt, g: e1_out[tt // 2].ap().rearrange("(k p) t -> p k t", p=128)[:, :, ds(g * 256 + (tt % 2) * 128, 128)],
                   "e1o", xres, h1d.ap(), wout0)
        if STOP not in ("A", "B"):
            P.new_phase()
            phase_attn()
        if STOP not in ("A", "B", "C"):
            P.new_phase()
            phase_post(1, 8, False,
                   lambda tt, g: e3_out[tt].ap().rearrange("(k p) t -> p k t", p=128)[:, :, ds(g * 128, 128)],
                   "e3o", h1d.ap(), hout, wout1)
        P.emit()
    return nc


def ret_consts(hd):
    dk, C = 256, 128
    lg = np.float32(np.log1p(-(2.0 ** (-5.0 - hd))))
    idx = np.arange(C, dtype=np.float32)
    diff = idx[:, None] - idx[None, :]
    dmask = np.where(diff >= 0, np.exp(lg * np.maximum(diff, 0.0)), 0.0).astype(np.float32)
    dmT = np.ascontiguousarray(dmask.T) * np.float32(dk ** -0.5)
    qdec = np.exp(lg * (idx + 1.0)).astype(np.float32)
    kdec = (np.exp(lg * (C - 1.0 - idx)) * np.float32(dk ** -0.5)).astype(np.float32)
    cdec = np.float32(np.exp(lg * C))
    return dict(dm=dmT.astype(np.float32), qd=np.tile(qdec[None, :], (128, 1)).astype(np.float32),
                kd=kdec[:, None].astype(np.float32), cd=np.full((128, 1), cdec, np.float32))


def ret_tables():
    inv = (10000.0 ** (-np.arange(0, 256, 2, dtype=np.float32) / 256)).astype(np.float32)
    ang = np.arange(S, dtype=np.float32)[None, :] * inv[:, None]
    return np.cos(ang).astype(np.float32), np.sin(ang).astype(np.float32)


def attn_consts():
    ind = np.zeros((64, 32 * 128), np.float32)
    for kt in range(32):
        for key in range(128):
            ind[2 * kt + key // 64, kt * 128 + key] = 1.0
    kk = np.arange(128)[:, None]; qq = np.arange(128)[None, :]
    caus = np.where(kk > qq, NEGB, 0.0).astype(np.float32)
    anti = np.where(kk <= qq, NEGB, 0.0).astype(np.float32)
    caus4 = np.tile(caus, (1, 4)); anti4 = np.tile(anti, (1, 4))
    cmk = np.zeros((128, 16, 2, 128), np.float32)
    m = np.arange(128)[:, None]; tp = np.arange(128)[None, :]
    for p in range(16):
        cmk[:, p, 0, :] = (16 * (m - 8 * p) + 31 <= tp)
        cmk[:, p, 1, :] = (16 * (m - 128 - 8 * p) + 31 <= tp)
    ci = np.arange(512)[:, None]; sj = np.arange(128)[None, :]
    ov = ((ci * 16 < (sj + 1) * 64) & (ci * 16 + 32 > sj * 64) & (ci < 511)).astype(np.float32)
    bt = np.zeros((128, 256), np.float32)
    for t in range(128):
        cr = 1 if t >= 64 else 0
        for c in range(256):
            d = c - 128
            if d == cr or d == cr - 1:
                bt[t, c] = 1000.0
            elif d > cr:
                bt[t, c] = -1e30
    return dict(ind=ind, caus4=caus4, anti4=anti4, cmk=cmk.reshape(128, -1), ov=ov, bt=bt)


_CACHE = {}


def kernel(**inputs):
    inp = {k: np.ascontiguousarray(np.asarray(v, dtype=np.float32)) for k, v in inputs.items()}
    x = inp["x"]
    B, S_, D = x.shape
    T = B * S_
    cores = list(range(8))
    if "nc" not in _CACHE:
        _CACHE["nc"] = build_fused()
    nc = _CACHE["nc"]
    w_in = inp["ret_w_in"][0]
    cosT, sinT = ret_tables()
    sel = np.zeros((16, 16 * 128), np.float32)
    for e in range(16):
        sel[e, e * 128:(e + 1) * 128] = 1.0
    pos = (np.arange(T) % S_).astype(np.float32)
    inv = (10000.0 ** (-np.arange(0, 64, 2, dtype=np.float32) / 64)).astype(np.float32)
    ang = pos[:, None] * inv[None, :]
    cs_full = np.concatenate([np.cos(ang), np.sin(ang)], 1).astype(np.float32)
    win = inp["nsa_w_in"][0]
    perm = np.array([(g * 4 + r) * 64 + d for r in range(4) for g in range(4) for d in range(64)])
    wp = np.ascontiguousarray(np.concatenate([inp["nsa_w_kv"], win[:, perm], win[:, 1024:1072]], 1))
    lnp = np.ascontiguousarray(np.stack([inp["ln_mix_g"][0], inp["ln_mix_b"][0], inp["ln_ffn_g"][0], inp["ln_ffn_b"][0],
                                         inp["ln_mix_g"][1], inp["ln_mix_b"][1], inp["ln_ffn_g"][1], inp["ln_ffn_b"][1]], 0))
    pe2 = lambda pe: np.ascontiguousarray(pe.reshape(16, 2, 64).transpose(1, 2, 0).reshape(128, 16))
    shared = dict(cosT=cosT, sinT=sinT, ident=np.eye(128, dtype=np.float32), sel=sel,
                  wout0=inp["ret_w_out"][0], wout1=inp["nsa_w_out"][0], lnp=lnp,
                  rw=inp["router_w"], rbias=inp["router_b"][None],
                  wg=inp["moe_w_gate"], wu=inp["moe_w_up"], wd=inp["moe_w_down"], wp=wp,
                  w1k=inp["cmp_k_w1"], w2k=inp["cmp_k_w2"], w1v=inp["cmp_v_w1"], w2v=inp["cmp_v_w2"],
                  pek=pe2(inp["cmp_pe_k"]), pev=pe2(inp["cmp_pe_v"]))
    shared.update(attn_consts())
    xf = x.reshape(T, D)
    xTs = [np.ascontiguousarray(x[b].T) for b in range(B)]
    maps = []
    for c in cores:
        b, hd = c // 4, c % 4
        sl = slice(c * NT, (c + 1) * NT)
        wr = np.ascontiguousarray(np.concatenate([
            w_in[:, hd * 256:(hd + 1) * 256], w_in[:, 1024 + hd * 256:1024 + (hd + 1) * 256],
            w_in[:, 2048 + hd * 512:2048 + (hd + 1) * 512], w_in[:, 4096 + hd * 512:4096 + (hd + 1) * 512]], 1))
        m = dict(shared)
        m.update(xT=xTs[b], wr=wr, xres=np.ascontiguousarray(xf[sl]), cs=np.ascontiguousarray(cs_full[sl]))
        m.update(ret_consts(hd))
        maps.append(m)
    res = run_bass_kernel_spmd(nc, maps, core_ids=cores)
    _LAST["res"] = res
    out = np.concatenate([r["hout"] for r in res.results], 0).reshape(B, S_, D)
    return out.astype(np.float32)
```
